# Optimizing a Trainium2 kernel written in Bass

```python
import jax, jax.numpy as jnp
from jax import lax
import numpy as np

D_MODEL = 1024
BATCH = 32
SEQ = 2048
DEPTH = 1

D_CONV = 512
CONV_GROUPS = 8
CONV_WIDTH = 3
HG_HEADS = 4
HG_KD = 128
HG_VD = 128
D_HG = HG_HEADS * HG_KD
D_MIX = D_CONV + D_HG
D_IN = 3 * D_CONV + 4 * D_HG
CHUNK = 32
PEER_HEADS = 8
N_KEYS = 128
N_EXPERTS = N_KEYS * N_KEYS
D_QUERY = 256
HALF_Q = D_QUERY // 2
PEER_TOPK = 16
TOKEN_BLOCK = 128
EPS = 1e-6

kernel_name = "hymba_conv_hgrn2_peer_block"


def rmsnorm(x, gain):
    x32 = x.astype(jnp.float32)
    y = x32 * lax.rsqrt(jnp.mean(x32 * x32, axis=-1, keepdims=True) + EPS)
    return (y * gain.astype(jnp.float32)).astype(x.dtype)


def group_rmsnorm(x, gain, groups):
    shp = x.shape
    xg = x.reshape(shp[:-1] + (groups, shp[-1] // groups)).astype(jnp.float32)
    y = xg * lax.rsqrt(jnp.mean(xg * xg, axis=-1, keepdims=True) + EPS)
    return (y.reshape(shp) * gain.astype(jnp.float32)).astype(x.dtype)


def short_conv_mixer(c_gate, h, b_gate, conv_w, gain):
    u = c_gate * h
    s = u.shape[1]
    up = jnp.pad(u, ((0, 0), (CONV_WIDTH - 1, 0), (0, 0)))
    y = conv_w[0] * up[:, 0:s]
    for j in range(1, CONV_WIDTH):
        y = y + conv_w[j] * up[:, j:j + s]
    y = b_gate * y
    return group_rmsnorm(y, gain, CONV_GROUPS)


def hgrn2_mixer(q, f_pre, v, g, lb, gain):
    f32 = jnp.float32
    bsz, s, _ = q.shape
    n_chunks = s // CHUNK

    def heads(t):
        return t.reshape(bsz, n_chunks, CHUNK, HG_HEADS, -1).transpose(0, 3, 1, 2, 4)

    qh = heads(jax.nn.silu(q.astype(f32)) * (HG_KD ** -0.5))
    f = lb + (1.0 - lb) * jax.nn.sigmoid(f_pre.astype(f32))
    kh = heads(1.0 - f)
    logf = heads(jnp.log(f))
    vh = heads(v.astype(f32))
    G = jnp.cumsum(logf, axis=3)
    G_last = G[:, :, :, -1:, :]
    q_dec = qh * jnp.exp(G)
    k_dec = kh * jnp.exp(-G)
    causal = jnp.tril(jnp.ones((CHUNK, CHUNK), dtype=bool))
    A = jnp.einsum('bhnck,bhnsk->bhncs', q_dec, k_dec)
    A = jnp.where(causal, A, 0.0)
    o_intra = jnp.einsum('bhncs,bhnsv->bhncv', A, vh)
    k_end = kh * jnp.exp(G_last - G)
    decay = jnp.exp(G_last[:, :, :, 0, :])

    def step(state, xs):
        dec_n, k_n, v_n, q_n = xs
        o_n = jnp.einsum('bhck,bhkv->bhcv', q_n, state)
        state = dec_n[..., None] * state + jnp.einsum('bhck,bhcv->bhkv', k_n, v_n)
        return state, o_n

    state0 = jnp.zeros((bsz, HG_HEADS, HG_KD, HG_VD), f32)
    _, o_inter = lax.scan(step, state0, (jnp.moveaxis(decay, 2, 0), jnp.moveaxis(k_end, 2, 0),
                                         jnp.moveaxis(vh, 2, 0), jnp.moveaxis(q_dec, 2, 0)))
    o = o_intra + jnp.moveaxis(o_inter, 0, 2)
    o = o * lax.rsqrt(jnp.mean(o * o, axis=-1, keepdims=True) + EPS) * gain.astype(f32)
    o = o * jax.nn.silu(heads(g.astype(f32)))
    o = o.transpose(0, 2, 3, 1, 4).reshape(bsz, s, HG_HEADS * HG_VD)
    return o.astype(q.dtype)


def peer_ffn(x, wq, keys, u_tab, v_tab):
    bsz, s, d = x.shape
    t = bsz * s
    xt = x.reshape(t, d)
    q = (xt @ wq).reshape(t, PEER_HEADS, 2, HALF_Q)
    scores = jnp.einsum('thpd,hpnd->thpn', q, keys).astype(jnp.float32)
    s_top, i_top = lax.top_k(scores, PEER_TOPK)
    cand = (s_top[:, :, 0, :, None] + s_top[:, :, 1, None, :]).reshape(t, PEER_HEADS, PEER_TOPK * PEER_TOPK)
    c_score, c_idx = lax.top_k(cand, PEER_TOPK)
    i1 = jnp.take_along_axis(i_top[:, :, 0], c_idx // PEER_TOPK, axis=-1)
    i2 = jnp.take_along_axis(i_top[:, :, 1], c_idx % PEER_TOPK, axis=-1)
    expert = i1 * N_KEYS + i2
    gates = jax.nn.softmax(c_score, axis=-1)
    n_blocks = t // TOKEN_BLOCK

    def block(args):
        xb, eb, gb = args
        u = jnp.take(u_tab, eb, axis=0)
        a = jax.nn.gelu(jnp.einsum('td,thkd->thk', xb, u).astype(jnp.float32), approximate=False)
        w = (gb * a).astype(xb.dtype)
        vv = jnp.take(v_tab, eb, axis=0)
        return jnp.einsum('thk,thkd->td', w, vv)

    y = lax.map(block, (xt.reshape(n_blocks, TOKEN_BLOCK, d),
                        expert.reshape(n_blocks, TOKEN_BLOCK, PEER_HEADS, PEER_TOPK),
                        gates.reshape(n_blocks, TOKEN_BLOCK, PEER_HEADS, PEER_TOPK)))
    return y.reshape(bsz, s, d)


def setup_inputs(seed: int = 0) -> dict:
    key = jax.random.key(seed)
    ks = jax.random.split(key, 16)
    f32 = jnp.float32
    nrm = lambda k, shp: jax.random.normal(k, shp, f32)
    return {
        "x": nrm(ks[0], (BATCH, SEQ, D_MODEL)),
        "norm_mix": 1.0 + 0.02 * nrm(ks[1], (DEPTH, D_MODEL)),
        "w_in": nrm(ks[2], (DEPTH, D_MODEL, D_IN)) * D_MODEL ** -0.5,
        "conv_w": nrm(ks[3], (DEPTH, CONV_WIDTH, D_CONV)) * CONV_WIDTH ** -0.5,
        "conv_gain": 1.0 + 0.02 * nrm(ks[4], (DEPTH, D_CONV)),
        "hg_lb_logits": 0.1 * nrm(ks[5], (DEPTH + 1, D_HG)),
        "hg_gain": 1.0 + 0.02 * nrm(ks[6], (DEPTH, HG_VD)),
        "w_out": nrm(ks[7], (DEPTH, D_MIX, D_MODEL)) * D_MIX ** -0.5,
        "norm_ffn": 1.0 + 0.02 * nrm(ks[8], (DEPTH, D_MODEL)),
        "peer_wq": nrm(ks[9], (DEPTH, D_MODEL, PEER_HEADS * D_QUERY)) * D_MODEL ** -0.5,
        "peer_keys": nrm(ks[10], (DEPTH, PEER_HEADS, 2, N_KEYS, HALF_Q)) * HALF_Q ** -0.5,
        "peer_u": nrm(ks[11], (DEPTH, N_EXPERTS, D_MODEL)) * D_MODEL ** -0.5,
        "peer_v": nrm(ks[12], (DEPTH, N_EXPERTS, D_MODEL)) * PEER_HEADS ** -0.5,
        "norm_f": 1.0 + 0.02 * nrm(ks[13], (D_MODEL,)),
    }


def reference(x, norm_mix, w_in, conv_w, conv_gain, hg_lb_logits, hg_gain, w_out,
              norm_ffn, peer_wq, peer_keys, peer_u, peer_v, norm_f):
    lb_all = jnp.cumsum(jax.nn.softmax(hg_lb_logits.astype(jnp.float32), axis=0), axis=0)
    splits = [D_CONV, 2 * D_CONV, 3 * D_CONV, 3 * D_CONV + D_HG, 3 * D_CONV + 2 * D_HG, 3 * D_CONV + 3 * D_HG]
    for l in range(DEPTH):
        h = rmsnorm(x, norm_mix[l])
        proj = h @ w_in[l]
        c_gate, hc, b_gate, q, f_pre, v, g = jnp.split(proj, splits, axis=-1)
        conv_out = short_conv_mixer(c_gate, hc, b_gate, conv_w[l], conv_gain[l])
        hg_out = hgrn2_mixer(q, f_pre, v, g, lb_all[l], hg_gain[l])
        mix = jnp.concatenate([conv_out, hg_out], axis=-1)
        x = x + mix @ w_out[l]
        h2 = rmsnorm(x, norm_ffn[l])
        x = x + peer_ffn(h2, peer_wq[l], peer_keys[l], peer_u[l], peer_v[l])
    return rmsnorm(x, norm_f)
```

```python
import numpy as np
from contextlib import ExitStack
import concourse.bass as bass
import concourse.mybir as mybir
from concourse.bass_utils import run_bass_kernel_spmd

F32 = mybir.dt.float32
BF16 = mybir.dt.bfloat16
U32 = mybir.dt.uint32
AF = mybir.ActivationFunctionType
ALU = mybir.AluOpType
AX = mybir.AxisListType

D = 1024
DIN = 3584
NE = 16384
EPS = 1e-6
N_CORES = 8
TT = 256
NSUB = TT // 128
GI = 1
NS = 4


class Prog:
    def __init__(self):
        self.ops = []
        self.cap = None
        self.defer = {}

    def op(self, eng, fn, r=(), w=(), dma=None, cost=None, defer=0):
        rec = (eng, fn, tuple(r), tuple(w), dma)
        if defer:
            self.defer[id(rec)] = defer
        if self.cap is not None:
            self.cap.append((rec, 0.3 if cost is None else cost))
        else:
            self.ops.append(rec)

    def emit(self, nc, stack):
        ops = self.ops
        n = len(ops)
        last_w, readers = {}, {}
        deps = [None] * n
        for i, (eng, fn, r, w, dma) in enumerate(ops):
            d = set()
            for k in r:
                if k in last_w:
                    d.add(last_w[k])
            for k in w:
                if k in last_w:
                    d.add(last_w[k])
                d.update(readers.get(k, ()))
            d.discard(i)
            deps[i] = d
            for k in r:
                readers.setdefault(k, []).append(i)
            for k in w:
                last_w[k] = i
                readers[k] = []

        def prod(j):
            return ('dma', ops[j][4]) if ops[j][4] is not None else ('eng', ops[j][0])

        needed = [False] * n
        red = [None] * n
        for i in range(n):
            m = {}
            for j in deps[i]:
                p = prod(j)
                if p == ('eng', 'pe') and ops[i][0] == 'pe' and ops[i][4] is None:
                    continue
                if p not in m or m[p] < j:
                    m[p] = j
            red[i] = m
            for j in m.values():
                needed[j] = True
        cnt, val = {}, [0] * n
        for j in range(n):
            p = prod(j)
            if p[0] == 'dma':
                cnt[p] = cnt.get(p, 0) + 16
                val[j] = cnt[p]
            elif needed[j]:
                cnt[p] = cnt.get(p, 0) + 1
                val[j] = cnt[p]
        sems = {}
        for idx, p in enumerate(sorted(cnt.keys(), key=str)):
            sems[p] = stack.enter_context(nc.semaphore("sm%d" % idx))
        self.n_sems = len(sems)
        block = stack.enter_context(nc.Block())

        def make(engname):
            def body(e):
                waited = {}
                for i, (eng, fn, r, w, dma) in enumerate(ops):
                    if eng != engname:
                        continue
                    for p, j in red[i].items():
                        v = val[j]
                        if waited.get(p, 0) < v:
                            e.wait_ge(sems[p], v)
                            waited[p] = v
                    if fn is None:
                        continue
                    ins = fn(e)
                    if dma is not None:
                        ins.then_inc(sems[prod(i)], 16)
                    elif needed[i]:
                        ins.then_inc(sems[prod(i)], 1)
            return body

        block.tensor(make('pe'))
        block.scalar(make('act'))
        block.vector(make('dve'))
        block.gpsimd(make('pool'))
        block.sync(make('sp'))


def build_nc(n_seq, seq_len, debug=()):
    ntok = n_seq * seq_len
    assert seq_len % TT == 0
    nblk = ntok // TT
    blk_per_seq = seq_len // TT
    nc = bass.Bass("TRN2", target_bir_lowering=False)
    P = Prog()
    st = ExitStack()

    def dram_in(name, shape, dt=F32):
        return nc.dram_tensor(name, list(shape), dt, kind="ExternalInput").ap()

    x_d = dram_in("x", [ntok, D])
    gm_d = dram_in("gm_pk", [128, 8])
    gf_d = dram_in("gf_pk", [128, 8])
    gfin_d = dram_in("gfin_bc", [128, D])
    NWG = 13
    wcat_d = dram_in("wcat", [NWG * 128, 4096])
    cw_d = dram_in("cw", [128, 4, 3])
    cgain_d = dram_in("cgain", [128, 4])
    lbl_d = dram_in("lbl", [128, 4, 2])
    hgain_d = dram_in("hgain", [128, 1])
    kT_d = dram_in("kT", [128, 16, 128])
    NG = 128 // GI
    UT_d = dram_in("UT", [NG * 128, 8 * GI * 128])
    V_d = dram_in("V", [NG * 128, GI * D])
    out_d = nc.dram_tensor("out", [ntok, D], F32, kind="ExternalOutput").ap()
    wcatb_d = nc.dram_tensor("wcat_b", [NWG * 128, 4096], BF16, kind="Internal").ap()
    UTb_d = nc.dram_tensor("UT_b", [NG * 128, 8 * GI * 128], BF16, kind="Internal").ap()
    Vb_d = nc.dram_tensor("V_b", [NG * 128, GI * D], BF16, kind="Internal").ap()

    def sb(name, shape, dt=F32):
        return st.enter_context(nc.sbuf_tensor(name, list(shape), dt))

    PS = [st.enter_context(nc.psum_tensor("ps%d" % i, [128, 512], F32)) for i in range(8)]
    PK = ["ps%d" % i for i in range(8)]

    identF = sb("identF", [128, 128]); identB = sb("identB", [128, 128], BF16)
    iotaF = sb("iotaF", [128, 128]); iotaB = sb("iotaB", [128, 128], BF16)
    causal = sb("causal", [128, 128])
    onesdiv = sb("onesdiv", [128, 128]); grp64 = sb("grp64", [128, 128])
    ones_t = sb("ones_t", [128, 128])
    eps_t = sb("eps_t", [128, 1]); nhalf = sb("nhalf", [128, 1])
    gm_bc = sb("gm_pk_s", [128, 8]); gf_bc = sb("gf_pk_s", [128, 8]); gfin_bc = sb("gfin_bc_s", [128, D])
    cw = sb("cw_s", [128, 4, 3]); cgain = sb("cgain_s", [128, 4]); lbl = sb("lbl_s", [128, 4, 2])
    lb = sb("lb", [128, 4]); oml = sb("oml", [128, 4]); hgain = sb("hgain_s", [128, 1])
    Wst = sb("Wst", [128, 2, 8, 512], BF16)
    kT_sb = sb("kT_sb", [128, 16, 128], BF16)
    BIG = sb("BIG", [128, 32768], BF16)
    WT = BIG[:, 0:128 * TT].rearrange("p (t i) -> p t i", i=128)

    xres = sb("xres", [128, 2, NSUB, D])
    hb = sb("hb", [128, D], BF16)
    hT = sb("hT", [128, 8, 128], BF16)
    stat = sb("stat", [128, 8])
    v_sb = sb("v_sb", [128, 512], BF16)
    cgs = sb("cgs", [128, 4, 128]); hist = sb("hist", [128, 4, 2])
    uext = sb("uext", [128, 4, 130])
    ycv = sb("ycv", [128, 4, 128])
    rs = sb("rs", [128, 512])
    mixT = sb("mixT", [128, 8, 128], BF16)
    qs = sb("qs", [128, 4, 128]); ff = sb("ff", [128, 4, 128]); kh = sb("kh", [128, 4, 128])
    logf = sb("logf", [128, 4, 128]); Gt = sb("Gt", [128, 4, 128]); gs = sb("gs", [128, 4, 128])
    negref = sb("negref", [128, 4]); dec = sb("dec", [128, 4])
    qdT = sb("qdT", [128, 4, 128], BF16); kdT = sb("kdT", [128, 4, 128], BF16)
    qd0T = sb("qd0T", [128, 4, 128], BF16); keT = sb("keT", [128, 4, 128], BF16)
    AmT = sb("AmT", [128, 4, 128], BF16); kend_tm = sb("kend_tm", [128, 4, 128], BF16)
    S = sb("S", [128, 4, 128]); S_bf = sb("S_bf", [128, 4, 128], BF16)
    o2 = sb("o2", [128, 512])
    ytmp = rs[:].rearrange("p (a t) -> p a t", a=4); Ex = o2[:].rearrange("p (a t) -> p a t", a=4)
    h2T = sb("h2T", [128, 2, 8, TT], BF16)
    qT = sb("qT", [128, 16, 128], BF16)
    sc_sb = sb("sc_sb", [128, 16, 128])
    V16 = sb("V16", [128, 16, 16]); IX = sb("IX", [128, 16, 16], U32); IXf = sb("IXf", [128, 16, 16])
    cand = sb("cand", [128, 8, 256])
    CS = sb("CS", [128, 8, 16]); CP = sb("CP", [128, 8, 16], U32)
    csh = sb("csh", [128, 8, 16]); ee = csh; Zs = sb("Zs", [128, 8]); rz = sb("rz", [128, 8])
    Gg = sb("Gg", [128, 8, 16])
    r1u = sb("r1u", [128, 8, 16], U32); r2u = sb("r2u", [128, 8, 16], U32)
    r1f = sb("r1f", [128, 8, 16]); r2f = sb("r2f", [128, 8, 16])
    oh = cand[:].rearrange("p h (a b) -> p h a b", a=16)
    oh2 = sc_sb[:].rearrange("p a b -> p (a b)").rearrange("p (h a b) -> p h a b", h=8, a=16)
    I1f = sb("I1f", [128, 8, 16]); I2f = sb("I2f", [128, 8, 16])
    slots = sb("slots", [128, 3, TT])
    NOH = 4
    P2b = sb("P2b", [128, NOH, 128], BF16); GPb = sb("GPb", [128, NOH, 128], BF16)
    ge = sb("ge", [128, 4, TT], BF16); WA = sb("WA", [128, 4, TT], BF16)
    Ubuf = sb("Ubuf", [128, NS, 8, GI * 128], BF16); Vbuf = sb("Vbuf", [128, NS, GI, D], BF16)

    dbg_outs = {}

    def dump(name, ap, key, shape, dt=F32):
        if name not in debug:
            return
        cntn = sum(1 for k in dbg_outs if k.startswith(name))
        nm = "%s_%d" % (name, cntn)
        dd = nc.dram_tensor("dbg_" + nm, list(shape), dt, kind="ExternalOutput").ap()
        dbg_outs[nm] = True
        P.op('sp', lambda e, dd=dd, ap=ap: e.dma_start(out=dd, in_=ap), r=[key], w=['dbgout_' + nm], dma='dbg')

    P.op('pool', lambda e: e.iota(iotaF[:], pattern=[[1, 128]], base=0, channel_multiplier=0,
                                  allow_small_or_imprecise_dtypes=True), w=['iotaF'])
    P.op('pool', lambda e: e.memset(identF[:], 1.0), w=['identF'])
    P.op('pool', lambda e: e.affine_select(out=identF[:], in_=identF[:], pattern=[[1, 128]],
                                           compare_op=ALU.is_equal, fill=0.0, base=0, channel_multiplier=-1),
         r=['identF'], w=['identF'])
    P.op('pool', lambda e: e.memset(causal[:], 1.0), w=['causal'])
    P.op('pool', lambda e: e.affine_select(out=causal[:], in_=causal[:], pattern=[[1, 128]],
                                           compare_op=ALU.is_ge, fill=0.0, base=0, channel_multiplier=-1),
         r=['causal'], w=['causal'])
    P.op('pool', lambda e: e.memset(onesdiv[:], 1.0 / 128), w=['onesdiv'])
    P.op('pool', lambda e: e.memset(grp64[:], 0.0), w=['grp64'])
    P.op('pool', lambda e: e.memset(grp64[0:64, 0:64], 1.0 / 64), r=['grp64'], w=['grp64'])
    P.op('pool', lambda e: e.memset(grp64[64:128, 64:128], 1.0 / 64), r=['grp64'], w=['grp64'])
    P.op('pool', lambda e: e.memset(ones_t[:], 1.0), w=['ones_t'])
    P.op('pool', lambda e: e.memset(eps_t[:], EPS), w=['eps_t'])
    P.op('pool', lambda e: e.memset(nhalf[:], -0.5), w=['nhalf'])
    P.op('dve', lambda e: e.tensor_copy(out=identB[:], in_=identF[:]), r=['identF'], w=['identB'])
    P.op('dve', lambda e: e.tensor_copy(out=iotaB[:], in_=iotaF[:]), r=['iotaF'], w=['iotaB'])

    for nm, dd, ss_ in (("gm", gm_d, gm_bc), ("gf", gf_d, gf_bc), ("gfin", gfin_d, gfin_bc), ("cw", cw_d, cw),
                        ("cgain", cgain_d, cgain), ("lbl", lbl_d, lbl), ("hgain", hgain_d, hgain)):
        P.op('sp', lambda e, dd=dd, ss_=ss_: e.dma_start(out=ss_[:], in_=dd), w=[nm], dma='par_' + nm)
    P.op('dve', lambda e: e.tensor_tensor(out=stat[:, 0:4], in0=lbl[:, :, 0], in1=lbl[:, :, 1], op=ALU.subtract),
         r=['lbl'], w=['stat'])
    P.op('act', lambda e: e.activation(out=lb[:], in_=stat[:, 0:4], func=AF.Sigmoid), r=['stat'], w=['lb'])
    P.op('dve', lambda e: e.tensor_scalar(out=oml[:], in0=lb[:], scalar1=-0.5, scalar2=0.5, op0=ALU.mult, op1=ALU.add),
         r=['lb'], w=['oml'])
    P.op('dve', lambda e: e.tensor_tensor(out=lb[:], in0=lb[:], in1=oml[:], op=ALU.add), r=['lb', 'oml'], w=['lb'])

    for k in range(NWG):
        P.op('pool', lambda e, k=k: e.dma_start(out=wcatb_d[k * 128:(k + 1) * 128, :], in_=wcat_d[k * 128:(k + 1) * 128, :]),
             w=['wcb%d' % k], dma='cast_w')
    P.op('pool', lambda e: e.dma_start(out=kT_sb[:], in_=kT_d), w=['kT_sb'], dma='cast_kT')
    NCU = 32
    RPC = NG * 128 // NCU
    for c in range(NCU):
        P.op('pool', lambda e, c=c: e.dma_start(out=UTb_d[c * RPC:(c + 1) * RPC, :], in_=UT_d[c * RPC:(c + 1) * RPC, :]),
             w=['UTb%d' % c], dma='cast_U')
        P.op('pool', lambda e, c=c: e.dma_start(out=Vb_d[c * RPC:(c + 1) * RPC, :], in_=V_d[c * RPC:(c + 1) * RPC, :]),
             w=['Vb%d' % c], dma='cast_V')
    ALL_UTB = ['UTb%d' % c for c in range(NCU)]
    ALL_VB = ['Vb%d' % c for c in range(NCU)]

    SC = 128.0 ** -0.5

    def rmsnorm_to_bf16(src_ap, src_keys, gain_bc, gain_key):
        P.op('dve', lambda e: e.tensor_tensor(out=hb[:], in0=src_ap, in1=src_ap, op=ALU.mult), r=src_keys, w=['hb'], cost=1.2)
        P.op('dve', lambda e: e.reduce_sum(out=stat[:, 4:5], in_=hb[:], axis=AX.X), r=['hb'], w=['stat'], cost=1.2)
        P.op('dve', lambda e: e.tensor_scalar(out=stat[:, 5:6], in0=stat[:, 4:5], scalar1=1.0 / D, scalar2=EPS, op0=ALU.mult, op1=ALU.add),
             r=['stat'], w=['stat'], cost=0.15)
        P.op('pool', lambda e: e.tensor_tensor(out=stat[:, 6:7], in0=stat[:, 5:6], in1=nhalf[:], op=ALU.pow),
             r=['stat', 'nhalf'], w=['stat'], cost=0.5)

    BX, BY = 6, 7
    wld = [0]

    pending = {}

    def issue_w(grp):
        slot = wld[0] % 2
        wld[0] += 1
        P.op('pool', lambda e: e.dma_start(out=Wst[:, slot, :, :].rearrange("p k n -> p (k n)"), in_=wcatb_d[grp * 128:(grp + 1) * 128, :]),
             r=['wcb%d' % grp], w=['Wst%d' % slot], dma='W%d' % slot, cost=0.0)
        pending[grp] = slot

    def load_w(grp):
        if grp not in pending:
            issue_w(grp)
        slot = pending.pop(grp)
        issue_w((grp + 1) % NWG)
        return slot

    def pre(b, sub):
        cur = b % 2
        t0 = b * TT + sub * 128
        first = (b % blk_per_seq == 0) and sub == 0
        xr = xres[:, cur, sub, :]
        xk = 'xres%d_%d' % (cur, sub)
        hk = 'h2T%d' % cur
        tsl = slice(sub * 128, (sub + 1) * 128)
        P.op('pool', lambda e: e.dma_start(out=xr, in_=x_d[t0:t0 + 128, :]), w=[xk], dma='xload%d_%d' % (cur, sub), cost=0.0)
        if first:
            P.op('pool', lambda e: e.memset(S[:], 0.0), w=['S'], cost=0.0)
            P.op('pool', lambda e: e.memset(S_bf[:], 0.0), w=['S_bf'], cost=0.0)
            P.op('pool', lambda e: e.memset(uext[:, :, 0:2], 0.0), w=['uext'], cost=0.0)
        rmsnorm_to_bf16(xr, [xk], None, None)
        P.op('dve', lambda e: e.tensor_scalar(out=hb[:], in0=xr, scalar1=stat[:, 6:7], scalar2=None, op0=ALU.mult),
             r=[xk, 'stat'], w=['hb'], cost=1.2)
        psx = PS[BX][:].bitcast(BF16)
        psy = PS[BY][:].bitcast(BF16)
        for k in range(8):
            P.op('pe', lambda e, k=k: e.transpose(psx[:, k * 128:(k + 1) * 128], hb[:, k * 128:(k + 1) * 128], identB[:]),
                 r=['hb', 'identB'], w=[PK[BX]], cost=0.12, defer=(4 if k == 0 else 0))
        for k in range(8):
            P.op('act', lambda e, k=k: e.activation(out=hT[:, k, :], in_=psx[:, k * 128:(k + 1) * 128], func=AF.Copy, scale=gm_bc[:, k:k + 1]),
                 r=[PK[BX], 'gm'], w=['hT'], cost=0.3)
        ws = load_w(0)
        for k in range(8):
            P.op('pe', lambda e, k=k: e.matmul(PS[BY][:], hT[:, k, :], Wst[:, ws, k, :], start=(k == 0), stop=(k == 7)),
                 r=['hT', 'Wst%d' % ws], w=[PK[BY]], cost=0.27)
        P.op('act', lambda e: e.activation(out=v_sb[:], in_=PS[BY][:], func=AF.Copy), r=[PK[BY]], w=['v_sb'], cost=0.6)

        def fm_group(grp, bank):
            wsl = load_w(grp)
            for j in range(4):
                for k in range(8):
                    P.op('pe', lambda e, k=k, j=j: e.matmul(PS[bank][:, j * 128:(j + 1) * 128], Wst[:, wsl, k, j * 128:(j + 1) * 128], hT[:, k, :],
                                                            start=(k == 0), stop=(k == 7)),
                         r=['hT', 'Wst%d' % wsl], w=[PK[bank]], cost=0.12)
        qs2, ff2, gs2 = [t[:].rearrange("p a t -> p (a t)") for t in (qs, ff, gs)]
        fm_group(1, BX)
        P.op('act', lambda e: e.activation(out=cgs[:].rearrange("p a t -> p (a t)"), in_=PS[BX][:], func=AF.Copy), r=[PK[BX]], w=['cgs'], cost=0.6)
        fm_group(2, BY)
        P.op('dve', lambda e: e.tensor_tensor(out=uext[:, :, 2:130], in0=PS[BY][:].rearrange("p (a t) -> p a t", a=4), in1=cgs[:], op=ALU.mult),
             r=[PK[BY], 'cgs'], w=['uext'], cost=0.7)
        for c in range(4):
            P.op('dve', lambda e, c=c: e.tensor_scalar(out=ycv[:, c, :], in0=uext[:, c, 0:128], scalar1=cw[:, c, 0:1],
                                                       scalar2=None, op0=ALU.mult), r=['uext', 'cw'], w=['ycv%d' % c], cost=0.25)
        for c in range(4):
            P.op('dve', lambda e, c=c: e.scalar_tensor_tensor(out=ytmp[:, c, :], in0=uext[:, c, 1:129], scalar=cw[:, c, 1:2],
                                                              in1=ycv[:, c, :], op0=ALU.mult, op1=ALU.add),
                 r=['uext', 'cw', 'ycv%d' % c], w=['yt%d' % c] + (['rs'] if c == 0 else []), cost=0.3)
        for c in range(4):
            P.op('dve', lambda e, c=c: e.scalar_tensor_tensor(out=ycv[:, c, :], in0=uext[:, c, 2:130], scalar=cw[:, c, 2:3],
                                                              in1=ytmp[:, c, :], op0=ALU.mult, op1=ALU.add),
                 r=['uext', 'cw', 'yt%d' % c], w=['ycv%d' % c], cost=0.3)
        YCV = ['ycv%d' % c for c in range(4)]
        YT = ['yt%d' % c for c in range(4)]
        P.op('dve', lambda e: e.tensor_copy(out=hist[:], in_=uext[:, :, 128:130]), r=['uext'], w=['hist'], cost=0.1)
        P.op('dve', lambda e: e.tensor_copy(out=uext[:, :, 0:2], in_=hist[:]), r=['hist'], w=['uext'], cost=0.1)
        fm_group(3, BX)
        ycv2 = ycv[:].rearrange("p a t -> p (a t)")
        P.op('dve', lambda e: e.tensor_tensor(out=ycv2, in0=PS[BX][:], in1=ycv2, op=ALU.mult), r=[PK[BX]] + YCV, w=['ycv'] + YCV, cost=0.7)
        P.op('dve', lambda e: e.tensor_tensor(out=o2[:], in0=ycv2, in1=ycv2, op=ALU.mult), r=['ycv'] + YCV, w=['o2'], cost=0.7)
        P.op('pe', lambda e: e.matmul(PS[BY][:], grp64[:], o2[:], start=True, stop=True), r=['grp64', 'o2'], w=[PK[BY]], cost=1.0)
        P.op('act', lambda e: e.activation(out=rs[:], in_=PS[BY][:], func=AF.Sqrt, bias=eps_t[:], scale=1.0),
             r=[PK[BY], 'eps_t'], w=['rs'] + YT, cost=0.6)
        P.op('dve', lambda e: e.reciprocal(out=rs[:], in_=rs[:]), r=['rs'], w=['rs'], cost=1.2)
        for c in range(4):
            P.op('dve', lambda e, c=c: e.scalar_tensor_tensor(out=mixT[:, c, :], in0=ycv[:, c, :], scalar=cgain[:, c:c + 1],
                                                              in1=rs[:, c * 128:(c + 1) * 128], op0=ALU.mult, op1=ALU.mult),
                 r=['ycv', 'ycv%d' % c, 'cgain', 'rs'], w=['mixT%d' % c], cost=0.3)
        fm_group(4, BX)
        P.op('act', lambda e: e.activation(out=qs2, in_=PS[BX][:], func=AF.Tanh, scale=0.5), r=[PK[BX]], w=['qs'], cost=0.6)
        P.op('dve', lambda e: e.scalar_tensor_tensor(out=qs2, in0=qs2, scalar=1.0, in1=PS[BX][:], op0=ALU.add, op1=ALU.mult),
             r=['qs', PK[BX]], w=['qs'], cost=0.7)
        fm_group(5, BY)
        P.op('act', lambda e: e.activation(out=ff2, in_=PS[BY][:], func=AF.Tanh, scale=0.5), r=[PK[BY]], w=['ff'], cost=0.6)
        fm_group(6, BX)
        P.op('act', lambda e: e.activation(out=gs2, in_=PS[BX][:], func=AF.Tanh, scale=0.5), r=[PK[BX]], w=['gs'], cost=0.6)
        P.op('dve', lambda e: e.scalar_tensor_tensor(out=gs2, in0=gs2, scalar=1.0, in1=PS[BX][:], op0=ALU.add, op1=ALU.mult),
             r=['gs', PK[BX]], w=['gs'], cost=0.7)
        for h in range(4):
            P.op('dve', lambda e, h=h: e.tensor_scalar(out=ff[:, h, :], in0=ff[:, h, :], scalar1=oml[:, h:h + 1],
                                                       scalar2=lb[:, h:h + 1], op0=ALU.mult, op1=ALU.add),
                 r=['ff', 'oml', 'lb'], w=['ff'], cost=0.3)
        kh2, lg2, Gt2 = [t[:].rearrange("p a t -> p (a t)") for t in (kh, logf, Gt)]
        Ex2 = o2[:]
        P.op('dve', lambda e: e.tensor_scalar(out=kh2, in0=ff2, scalar1=-1.0, scalar2=1.0, op0=ALU.mult, op1=ALU.add),
             r=['ff'], w=['kh'], cost=0.5)
        P.op('act', lambda e: e.activation(out=lg2, in_=ff2, func=AF.Ln), r=['ff'], w=['logf'], cost=0.6, defer=2)
        for h in range(4):
            P.op('dve', lambda e, h=h: e.tensor_tensor_scan(out=Gt[:, h, :], data0=ones_t[:], data1=logf[:, h, :], initial=0.0,
                                                            op0=ALU.mult, op1=ALU.add), r=['ones_t', 'logf'], w=['Gt%d' % h], cost=0.35)
        GT = ['Gt%d' % h for h in range(4)]
        P.op('dve', lambda e: e.tensor_scalar(out=negref[:], in0=Gt[:, :, 63], scalar1=-1.0, scalar2=None, op0=ALU.mult),
             r=GT, w=['negref'], cost=0.15)
        P.op('act', lambda e: e.activation(out=dec[:], in_=Gt[:, :, 127], func=AF.Exp), r=GT, w=['dec'], cost=0.25, defer=2)
        for h in range(4):
            P.op('act', lambda e, h=h: e.activation(out=qdT[:, h, :], in_=Gt[:, h, :], func=AF.Exp, bias=negref[:, h:h + 1], scale=1.0),
                 r=GT + ['negref'], w=['qdT'], cost=0.3)
        for h in range(4):
            P.op('act', lambda e, h=h: e.activation(out=kdT[:, h, :], in_=Gt[:, h, :], func=AF.Exp, bias=Gt[:, h, 63:64], scale=-1.0),
                 r=GT, w=['kdT'], cost=0.3)
        P.op('act', lambda e: e.activation(out=qd0T[:].rearrange("p a t -> p (a t)"), in_=Gt2, func=AF.Exp), r=GT, w=['qd0T'], cost=0.6)
        for h in range(4):
            P.op('act', lambda e, h=h: e.activation(out=keT[:, h, :], in_=Gt[:, h, :], func=AF.Exp, bias=Gt[:, h, 127:128], scale=-1.0),
                 r=GT, w=['keT'], cost=0.3)
        SCH = 0.5 * SC
        qdT2, kdT2, qd0T2, keT2 = [t[:].rearrange("p a t -> p (a t)") for t in (qdT, kdT, qd0T, keT)]
        P.op('dve', lambda e: e.scalar_tensor_tensor(out=qdT2, in0=qs2, scalar=SCH, in1=qdT2, op0=ALU.mult, op1=ALU.mult),
             r=['qs', 'qdT'], w=['qdT'], cost=0.7)
        P.op('dve', lambda e: e.tensor_tensor(out=kdT2, in0=kh2, in1=kdT2, op=ALU.mult), r=['kh', 'kdT'], w=['kdT'], cost=0.7)
        P.op('dve', lambda e: e.scalar_tensor_tensor(out=qd0T2, in0=qs2, scalar=SCH, in1=qd0T2, op0=ALU.mult, op1=ALU.mult),
             r=['qs', 'qd0T'], w=['qd0T'], cost=0.7)
        P.op('dve', lambda e: e.tensor_tensor(out=keT2, in0=kh2, in1=keT2, op=ALU.mult), r=['kh', 'keT'], w=['keT'], cost=0.7)
        for h in range(4):
            P.op('pe', lambda e, h=h: e.matmul(PS[BY][:, h * 128:(h + 1) * 128], kdT[:, h, :], qdT[:, h, :], start=True, stop=True),
                 r=['kdT', 'qdT'], w=[PK[BY]], cost=0.12)
        P.op('dve', lambda e: e.tensor_tensor(out=AmT[:], in0=PS[BY][:].rearrange("p (a t) -> p a t", a=4),
                                              in1=causal[:].unsqueeze(1).broadcast_to([128, 4, 128]), op=ALU.mult),
             r=[PK[BY], 'causal'], w=['AmT'], cost=0.7)
        for h in range(4):
            P.op('pe', lambda e, h=h: e.transpose(psx[:, h * 128:(h + 1) * 128], keT[:, h, :], identB[:]),
                 r=['keT', 'identB'], w=[PK[BX]], cost=0.12)
        P.op('act', lambda e: e.activation(out=kend_tm[:].rearrange("p a t -> p (a t)"), in_=psx[:, 0:512], func=AF.Copy),
             r=[PK[BX]], w=['kend_tm'], cost=0.6)
        for h in range(4):
            P.op('pe', lambda e, h=h: e.matmul(PS[BY][:, h * 128:(h + 1) * 128], v_sb[:, h * 128:(h + 1) * 128], AmT[:, h, :],
                                               start=True, stop=False), r=['v_sb', 'AmT'], w=[PK[BY]], cost=0.12)
            P.op('pe', lambda e, h=h: e.matmul(PS[BY][:, h * 128:(h + 1) * 128], S_bf[:, h, :], qd0T[:, h, :],
                                               start=False, stop=True), r=['S_bf', 'qd0T'], w=[PK[BY]], cost=0.12)
        for h in range(4):
            P.op('pe', lambda e, h=h: e.matmul(PS[BX][:, h * 128:(h + 1) * 128], kend_tm[:, h, :], v_sb[:, h * 128:(h + 1) * 128],
                                               start=True, stop=True), r=['kend_tm', 'v_sb'], w=[PK[BX]], cost=0.12)
        for h in range(4):
            P.op('dve', lambda e, h=h: e.scalar_tensor_tensor(out=S[:, h, :], in0=S[:, h, :], scalar=dec[:, h:h + 1],
                                                              in1=PS[BX][:, h * 128:(h + 1) * 128], op0=ALU.mult, op1=ALU.add),
                 r=['S', 'dec', PK[BX]], w=['S'], cost=0.3)
        P.op('dve', lambda e: e.tensor_copy(out=S_bf[:], in_=S[:]), r=['S'], w=['S_bf'], cost=0.5)
        P.op('act', lambda e: e.activation(out=o2[:], in_=PS[BY][:], func=AF.Square), r=[PK[BY]], w=['o2'], cost=0.6)
        P.op('pe', lambda e: e.matmul(PS[BX][:], onesdiv[:], o2[:], start=True, stop=True), r=['onesdiv', 'o2'], w=[PK[BX]], cost=1.0)
        P.op('act', lambda e: e.activation(out=rs[:], in_=PS[BX][:], func=AF.Sqrt, bias=eps_t[:], scale=1.0),
             r=[PK[BX], 'eps_t'], w=['rs'], cost=0.6)
        P.op('dve', lambda e: e.reciprocal(out=rs[:], in_=rs[:]), r=['rs'], w=['rs'], cost=1.2)
        P.op('dve', lambda e: e.scalar_tensor_tensor(out=rs[:], in0=PS[BY][:], scalar=hgain[:, 0:1], in1=rs[:],
                                                     op0=ALU.mult, op1=ALU.mult), r=[PK[BY], 'hgain', 'rs'], w=['rs'], cost=0.7)
        P.op('dve', lambda e: e.scalar_tensor_tensor(out=mixT[:, 4:8, :].rearrange("p a t -> p (a t)"), in0=rs[:], scalar=0.5, in1=gs2,
                                                     op0=ALU.mult, op1=ALU.mult), r=['rs', 'gs'], w=['mixT4'], cost=0.7)
        MIXK = ['mixT%d' % c for c in range(5)]
        dump('mixT', mixT[:], 'mixT4', [128, 8, 128], BF16)
        for half, bank in ((0, BY), (1, BX)):
            wsl = load_w(7 + half)
            for k in range(8):
                P.op('pe', lambda e, k=k, wsl=wsl, bank=bank: e.matmul(PS[bank][:], mixT[:, k, :], Wst[:, wsl, k, :], start=(k == 0), stop=(k == 7)),
                     r=MIXK + ['Wst%d' % wsl], w=[PK[bank]], cost=0.27)
            P.op('dve', lambda e, half=half, bank=bank: e.tensor_tensor(out=xr[:, half * 512:(half + 1) * 512], in0=PS[bank][:],
                                                                        in1=xr[:, half * 512:(half + 1) * 512], op=ALU.add),
                 r=[PK[bank], xk], w=[xk], cost=0.7)
        dump('x2', xr, xk, [128, D])
        rmsnorm_to_bf16(xr, [xk], None, None)
        P.op('dve', lambda e: e.tensor_scalar(out=hb[:], in0=xr, scalar1=stat[:, 6:7], scalar2=None, op0=ALU.mult),
             r=[xk, 'stat'], w=['hb'], cost=1.2)
        for k in range(8):
            P.op('pe', lambda e, k=k: e.transpose(psy[:, k * 128:(k + 1) * 128], hb[:, k * 128:(k + 1) * 128], identB[:]),
                 r=['hb', 'identB'], w=[PK[BY]], cost=0.12, defer=(3 if k == 0 else 0))
        for k in range(8):
            P.op('act', lambda e, k=k: e.activation(out=h2T[:, cur, k, tsl], in_=psy[:, k * 128:(k + 1) * 128],
                                                    func=AF.Copy, scale=gf_bc[:, k:k + 1]),
                 r=[PK[BY], 'gf'], w=[hk], cost=0.3)
        marks.append(len(P.cap) if P.cap is not None else -1)
        for c4 in range(4):
            bank = BX if c4 % 2 == 0 else BY
            wsl = load_w(9 + c4)
            for j in range(4):
                for k in range(8):
                    P.op('pe', lambda e, k=k, j=j, wsl=wsl, bank=bank: e.matmul(PS[bank][:, j * 128:(j + 1) * 128],
                                                                              Wst[:, wsl, k, j * 128:(j + 1) * 128], h2T[:, cur, k, tsl],
                                                                              start=(k == 0), stop=(k == 7)),
                         r=[hk, 'Wst%d' % wsl], w=[PK[bank]], cost=0.12)
            P.op('act', lambda e, c4=c4, bank=bank: e.activation(out=qT[:, 4 * c4:4 * c4 + 4, :].rearrange("p a t -> p (a t)"),
                                                                 in_=PS[bank][:], func=AF.Copy), r=[PK[bank]], w=['qT%d' % c4], cost=0.6)
        for c4 in range(4):
            bank = BX if c4 % 2 == 0 else BY
            for j in range(4):
                jj = 4 * c4 + j
                P.op('pe', lambda e, j=j, jj=jj, bank=bank: e.matmul(PS[bank][:, j * 128:(j + 1) * 128], qT[:, jj, :], kT_sb[:, jj, :],
                                                                     start=True, stop=True), r=['qT%d' % c4, 'kT_sb'], w=[PK[bank]], cost=0.12)
            P.op('act', lambda e, c4=c4, bank=bank: e.activation(out=sc_sb[:, 4 * c4:4 * c4 + 4, :].rearrange("p a t -> p (a t)"),
                                                                 in_=PS[bank][:], func=AF.Copy), r=[PK[bank]], w=['sc_sb'], cost=0.6)
        dump('sc', sc_sb[:], 'sc_sb', [128, 16, 128])
        marks.append(len(P.cap) if P.cap is not None else -1)
        scr1 = cand[:].rearrange("p h n -> p (h n)").rearrange("p (j n) -> p j n", j=16)
        tagsA = ['a%d' % j for j in range(16)]
        top16_batch([(sc_sb[:, j, :], V16[:, j, :], IX[:, j, :], scr1[:, j, :], tagsA[j]) for j in range(16)], 'sc_sb', ['cand'])
        KV16 = ['tv' + t for t in tagsA]; KIX = ['ti' + t for t in tagsA]; KS1 = ['ts' + t for t in tagsA]
        P.op('dve', lambda e: e.tensor_copy(out=IXf[:], in_=IX[:]), r=KIX, w=['IXf'])
        v1 = V16[:, 0::2, :].unsqueeze(3).broadcast_to([128, 8, 16, 16])
        v2 = V16[:, 1::2, :].unsqueeze(2).broadcast_to([128, 8, 16, 16])
        P.op('dve', lambda e: e.tensor_tensor(out=cand[:].rearrange("p h (a b) -> p h a b", a=16), in0=v1, in1=v2, op=ALU.add),
             r=KV16, w=['cand'] + KS1, cost=2.3)
        scr2 = sc_sb[:].rearrange("p j n -> p (j n)").rearrange("p (h n) -> p h n", h=8)
        tagsB = ['b%d' % h for h in range(8)]
        top16_batch([(cand[:, h, :], CS[:, h, :], CP[:, h, :], scr2[:, h, :], tagsB[h]) for h in range(8)], 'cand', ['sc_sb'])
        KCS = ['tv' + t for t in tagsB]; KCP = ['ti' + t for t in tagsB]; KS2 = ['ts' + t for t in tagsB]
        P.op('dve', lambda e: e.tensor_tensor(out=csh[:], in0=CS[:], in1=CS[:, :, 0:1].broadcast_to([128, 8, 16]), op=ALU.subtract),
             r=KCS, w=['csh'])
        P.op('act', lambda e: e.activation(out=ee[:], in_=csh[:], func=AF.Exp), r=['csh'], w=['csh'], defer=3)
        P.op('dve', lambda e: e.reduce_sum(out=Zs[:], in_=ee[:], axis=AX.X), r=['csh'], w=['Zs'])
        P.op('dve', lambda e: e.reciprocal(out=rz[:], in_=Zs[:]), r=['Zs'], w=['rz'])
        P.op('dve', lambda e: e.tensor_tensor(out=Gg[:], in0=ee[:], in1=rz[:].unsqueeze(2).broadcast_to([128, 8, 16]), op=ALU.mult),
             r=['csh', 'rz'], w=['Gg'])
        P.op('dve', lambda e: e.tensor_single_scalar(out=r1u[:], in_=CP[:], scalar=4, op=ALU.logical_shift_right), r=KCP, w=['r1u'])
        P.op('dve', lambda e: e.tensor_single_scalar(out=r2u[:], in_=CP[:], scalar=15, op=ALU.bitwise_and), r=KCP, w=['r2u'])
        P.op('dve', lambda e: e.tensor_copy(out=r1f[:], in_=r1u[:]), r=['r1u'], w=['r1f'])
        P.op('dve', lambda e: e.tensor_copy(out=r2f[:], in_=r2u[:]), r=['r2u'], w=['r2f'])
        io16 = iotaF[:, 0:16].unsqueeze(1).unsqueeze(1).broadcast_to([128, 8, 16, 16])
        for (rf, rk, par, If, Ik) in ((r1f, 'r1f', 0, I1f, 'I1f'), (r2f, 'r2f', 1, I2f, 'I2f')):
            P.op('dve', lambda e, rf=rf: e.tensor_tensor(out=oh, in0=rf[:].unsqueeze(3).broadcast_to([128, 8, 16, 16]), in1=io16,
                                                         op=ALU.is_equal), r=[rk, 'iotaF'], w=['cand'], cost=2.3)
            P.op('dve', lambda e, par=par: e.tensor_tensor(out=oh2, in0=oh,
                                                           in1=IXf[:, par::2, :].unsqueeze(2).broadcast_to([128, 8, 16, 16]),
                                                           op=ALU.mult), r=['cand', 'IXf'], w=['sc_sb'] + KS2, cost=2.3)
            P.op('dve', lambda e, If=If: e.reduce_sum(out=If[:], in_=oh2, axis=AX.X), r=['sc_sb'], w=[Ik], cost=2.3)
        dump('I1f', I1f[:], 'I1f', [128, 8, 16]); dump('I2f', I2f[:], 'I2f', [128, 8, 16]); dump('Gg', Gg[:], 'Gg', [128, 8, 16])
        for i, (src, sk) in enumerate(((I1f, 'I1f'), (I2f, 'I2f'), (Gg, 'Gg'))):
            P.op('pe', lambda e, i=i, src=src: e.transpose(PS[BX][:, i * 128:(i + 1) * 128], src[:].rearrange("p h k -> p (h k)"), identF[:]),
                 r=[sk, 'identF'], w=[PK[BX]])
        P.op('act', lambda e: e.activation(out=slots[:, :, tsl], in_=PS[BX][:, 0:384].rearrange("p (a t) -> p a t", a=3), func=AF.Copy),
             r=[PK[BX]], w=['slots'], cost=0.6)


    def top16_batch(lists, src_key, first_extra_w):
        for (src, va, ix, tmp, tag) in lists:
            P.op('dve', lambda e, src=src, va=va: e.max(out=va[:, 0:8], in_=src), r=[src_key], w=['tv' + tag])
        for (src, va, ix, tmp, tag) in lists:
            P.op('dve', lambda e, src=src, va=va, ix=ix: e.max_index(out=ix[:, 0:8], in_max=va[:, 0:8], in_values=src),
                 r=[src_key, 'tv' + tag], w=['ti' + tag])
        for n, (src, va, ix, tmp, tag) in enumerate(lists):
            P.op('dve', lambda e, src=src, va=va, tmp=tmp: e.match_replace(out=tmp, in_to_replace=va[:, 0:8], in_values=src, imm_value=-1e30),
                 r=[src_key, 'tv' + tag], w=['ts' + tag] + (first_extra_w if n == 0 else []))
        for (src, va, ix, tmp, tag) in lists:
            P.op('dve', lambda e, va=va, tmp=tmp: e.max(out=va[:, 8:16], in_=tmp), r=['ts' + tag], w=['tv' + tag])
        for (src, va, ix, tmp, tag) in lists:
            P.op('dve', lambda e, src=src, va=va, ix=ix: e.max_index(out=ix[:, 8:16], in_max=va[:, 8:16], in_values=src),
                 r=[src_key, 'tv' + tag], w=['ti' + tag])

    ldc = [0]

    def scatter(b):
        for t in range(TT):
            sl = t % NOH
            P.op('dve', lambda e, t=t, sl=sl: e.tensor_scalar(out=P2b[:, sl, :], in0=iotaB[:], scalar1=slots[:, 1, t:t + 1], scalar2=None,
                                                              op0=ALU.is_equal), r=['slots', 'iotaB'], w=['P2b%d' % sl])
            P.op('dve', lambda e, t=t, sl=sl: e.tensor_scalar(out=GPb[:, sl, :], in0=iotaB[:], scalar1=slots[:, 0, t:t + 1],
                                                              scalar2=slots[:, 2, t:t + 1], op0=ALU.is_equal, op1=ALU.mult),
                 r=['slots', 'iotaB'], w=['GPb%d' % sl])
            bank = 4 + (t // 4) % 2
            P.op('pe', lambda e, t=t, sl=sl, bank=bank: e.matmul(PS[bank][:, (t % 4) * 128:(t % 4 + 1) * 128], P2b[:, sl, :], GPb[:, sl, :],
                                                                 start=True, stop=True),
                 r=['P2b%d' % sl, 'GPb%d' % sl], w=[PK[bank]])
            if t % 4 == 3:
                tb = t - 3
                P.op('act', lambda e, tb=tb, bank=bank: e.activation(out=WT[:, tb:tb + 4, :].rearrange("p t i -> p (t i)"),
                                                                     in_=PS[bank][:], func=AF.Copy),
                     r=[PK[bank]], w=['BIG'])

    def dense(b, pre_ops, prev_fin=None):
        cur = b % 2
        hk = 'h2T%d' % cur
        ldcount = ldc[0]
        LAG = 3
        total = sum(c for _, c in pre_ops)
        state = {'i': 0, 'cum': 0.0}
        NSPREAD = 118

        lastw = {}
        prod_np = [-1] * len(pre_ops)
        prod_any = {}
        estep = [0] * len(pre_ops)
        for idx, (rec, c) in enumerate(pre_ops):
            eng, fn, r, w, dma = rec
            if id(rec) in P.defer:
                prod_any[idx] = max([lastw.get(k, -1) for k in r] + [-1])
            if eng == 'pe':
                best = -1
                for k in r:
                    j = lastw.get(k, -1)
                    if j >= 0 and pre_ops[j][0][0] != 'pe':
                        best = max(best, j)
                prod_np[idx] = best
            for k in w:
                lastw[k] = idx

        def emit_pre(step):
            tgt = total * min(1.0, (step + 1) / float(NSPREAD))
            last = step >= 127 + LAG
            start_i = state['i']
            while state['i'] < len(pre_ops) and (state['cum'] < tgt or last):
                rec, c = pre_ops[state['i']]
                if not last and rec[0] == 'pe' and prod_np[state['i']] >= start_i:
                    break
                if not last and state['i'] in prod_any:
                    pj = prod_any[state['i']]
                    if pj >= 0 and step < estep[pj] + P.defer[id(rec)]:
                        break
                P.ops.append(rec)
                estep[state['i']] = step
                state['cum'] += c
                state['i'] += 1

        def load_U(g):
            slot = (ldcount + g) % NS
            P.op('sp', lambda e: e.dma_start(out=Ubuf[:, slot, :, :].rearrange("p k n -> p (k n)"), in_=UTb_d[g * 128:(g + 1) * 128, :]),
                 r=ALL_UTB, w=['Ubuf%d' % slot], dma='U%d' % slot)

        def load_V(g):
            slot = (ldcount + g) % NS
            P.op('sp', lambda e: e.dma_start(out=Vbuf[:, slot, :, :].rearrange("p c d -> p (c d)"), in_=Vb_d[g * 128:(g + 1) * 128, :]),
                 r=ALL_VB, w=['Vbuf%d' % slot], dma='V%d' % slot)

        def u_step(i1):
            g, j = divmod(i1, GI)
            slot = (ldcount + g) % NS
            ab = 4 + i1 % 2
            aps = PS[ab][:, 0:TT]
            for k in range(8):
                P.op('pe', lambda e, k=k: e.matmul(aps, Ubuf[:, slot, k, j * 128:(j + 1) * 128], h2T[:, cur, k, :], start=(k == 0), stop=(k == 7)),
                     r=['Ubuf%d' % slot, hk], w=[PK[ab]])
            gsl = i1 % 4
            P.op('act', lambda e: e.activation(out=ge[:, gsl, :], in_=aps, func=AF.Gelu), r=[PK[ab]], w=['ge%d' % gsl])
            ws = i1 % 4
            P.op('pool', lambda e: e.tensor_tensor(out=WA[:, ws, :], in0=ge[:, gsl, :], in1=WT[:, :, i1], op=ALU.mult),
                 r=['ge%d' % gsl, 'BIG'], w=['WA%d' % ws])

        def v_step(i1):
            g, j = divmod(i1, GI)
            slot = (ldcount + g) % NS
            ws = i1 % 4
            for sub in range(NSUB):
                for half in range(2):
                    yb = sub * 2 + half
                    P.op('pe', lambda e, sub=sub, half=half, yb=yb: e.matmul(PS[yb][:], WA[:, ws, sub * 128:(sub + 1) * 128],
                                                                             Vbuf[:, slot, j, half * 512:(half + 1) * 512],
                                                                             start=(i1 == 0), stop=(i1 == 127)),
                         r=['WA%d' % ws, 'Vbuf%d' % slot], w=[PK[yb]])

        NGRP = 128 // GI
        for g0 in range(NS - 1):
            load_U(g0)
        for g0 in range(NS):
            load_V(g0)
        pend_stores = prev_fin() if prev_fin is not None else []
        for step in range(128 + LAG):
            if step < 128:
                if step % GI == 0 and step // GI + NS - 1 < NGRP:
                    load_U(step // GI + NS - 1)
                u_step(step)
            if step >= LAG:
                iv = step - LAG
                v_step(iv)
                if (iv + 1) % GI == 0 and iv // GI + NS < NGRP:
                    load_V(iv // GI + NS)
            if step == 4:
                for st_ in pend_stores:
                    st_()
            if step >= 4 or not pend_stores:
                emit_pre(step)
        ldc[0] = ldcount + 128 // GI
        for sub in range(NSUB):
            xk = 'xres%d_%d' % (cur, sub)
            xr = xres[:, cur, sub, :]
            for half in range(2):
                P.op('dve', lambda e, sub=sub, half=half, xr=xr: e.tensor_tensor(out=xr[:, half * 512:(half + 1) * 512],
                                                                                 in0=PS[sub * 2 + half][:],
                                                                                 in1=xr[:, half * 512:(half + 1) * 512], op=ALU.add),
                     r=[PK[sub * 2 + half], xk], w=[xk])

        def fin():
            stores = []
            for sub in range(NSUB):
                t0 = b * TT + sub * 128
                xk = 'xres%d_%d' % (cur, sub)
                xr = xres[:, cur, sub, :]
                rmsnorm_to_bf16(xr, [xk], None, None)
                P.op('dve', lambda e, xr=xr: e.scalar_tensor_tensor(out=xr, in0=xr, scalar=stat[:, 6:7], in1=gfin_bc[:],
                                                                    op0=ALU.mult, op1=ALU.mult), r=[xk, 'stat', 'gfin'], w=[xk])
                stores.append(lambda t0=t0, xr=xr, xk=xk: P.op('sp', lambda e: e.dma_start(out=out_d[t0:t0 + 128, :], in_=xr),
                                                               r=[xk], w=['outdram'], dma='ost'))
            return stores
        return fin

    marks = []

    def merge_by_cost(la, lb_):
        ta = sum(c for _, c in la) or 1.0
        tb = sum(c for _, c in lb_) or 1.0
        out, ia, ib, ca, cb = [], 0, 0, 0.0, 0.0
        while ia < len(la) or ib < len(lb_):
            if ib >= len(lb_) or (ia < len(la) and ca / ta <= cb / tb):
                out.append(la[ia]); ca += la[ia][1]; ia += 1
            else:
                out.append(lb_[ib]); cb += lb_[ib][1]; ib += 1
        return out

    def capture_pre(b):
        assert NSUB == 2
        P.cap = []
        del marks[:]
        pre(b, 0)
        s1 = len(P.cap)
        pre(b, 1)
        ops_, P.cap = P.cap, None
        h0, t0, h1, t1 = marks
        A, T0, B, H1, T1 = ops_[:t0], ops_[t0:s1], ops_[s1:h1], ops_[h1:t1], ops_[t1:]
        return A + merge_by_cost(T0, B) + H1 + T1

    for rec, _ in capture_pre(0):
        P.ops.append(rec)
    fin_prev = None
    for b in range(nblk):
        nxt = capture_pre(b + 1) if b + 1 < nblk else []
        scatter(b)
        fin_prev = dense(b, nxt, fin_prev)
    for st_ in fin_prev():
        st_()
    fin = ['outdram'] + ['dbgout_' + k for k in dbg_outs]
    P.op('sp', None, r=fin)
    P.emit(nc, st)
    nc._keep = st
    return nc, list(dbg_outs.keys())


def make_in_maps(inputs, n_cores, n_seq):
    g = lambda k: np.asarray(inputs[k], dtype=np.float32)
    x = g("x")
    bc = lambda v: np.ascontiguousarray(np.broadcast_to(v.reshape(1, D), (128, D)))
    w_in = g("w_in")[0]; w_out = g("w_out")[0]; wq = g("peer_wq")[0]
    cols = [w_in[:, 2560:3072], w_in[:, 0:512], w_in[:, 512:1024], w_in[:, 1024:1536], w_in[:, 1536:2048], w_in[:, 2048:2560],
            w_in[:, 3072:3584], w_out[:, 0:512], w_out[:, 512:1024]] + [wq[:, c * 512:(c + 1) * 512] for c in range(4)]
    wcat = np.ascontiguousarray(np.stack([c.reshape(8, 128, 512).transpose(1, 0, 2) for c in cols], axis=0)).reshape(13 * 128, 4096)
    common = {
        "gm_pk": np.ascontiguousarray(g("norm_mix")[0].reshape(8, 128).T), "gf_pk": np.ascontiguousarray(g("norm_ffn")[0].reshape(8, 128).T),
        "gfin_bc": bc(g("norm_f")),
        "wcat": wcat,
        "cw": np.ascontiguousarray(g("conv_w")[0].T.reshape(4, 128, 3).transpose(1, 0, 2)),
        "cgain": np.ascontiguousarray(g("conv_gain")[0].reshape(4, 128).T),
        "lbl": np.ascontiguousarray(g("hg_lb_logits").T.reshape(4, 128, 2).transpose(1, 0, 2)),
        "hgain": np.ascontiguousarray(g("hg_gain")[0].reshape(128, 1)),
        "kT": np.ascontiguousarray(g("peer_keys")[0].reshape(16, 128, 128).transpose(2, 0, 1)),
        "UT": np.ascontiguousarray(g("peer_u")[0].reshape(128 // GI, GI * 128, 8, 128).transpose(0, 3, 2, 1)).reshape(128 // GI * 128, 8 * GI * 128),
        "V": np.ascontiguousarray(g("peer_v")[0].reshape(128 // GI, GI, 128, D).transpose(0, 2, 1, 3)).reshape(128 // GI * 128, GI * D),
    }
    seq = x.shape[1]
    xs = x.reshape(n_cores, n_seq * seq, D)
    return [dict(common, x=np.ascontiguousarray(xs[c])) for c in range(n_cores)]


def kernel(**inputs):
    x = np.asarray(inputs["x"])
    B, Sq, _ = x.shape
    n_seq = B // N_CORES
    nc, _ = build_nc(n_seq, Sq)
    in_maps = make_in_maps(inputs, N_CORES, n_seq)
    res = run_bass_kernel_spmd(nc, in_maps, core_ids=list(range(N_CORES)))
    out = np.stack([np.asarray(r["out"]) for r in res.results], axis=0)
    return out.reshape(B, Sq, D).astype(np.float32)
```

```python
import numpy as np
from contextlib import ExitStack
import concourse.bass as bass
import concourse.mybir as mybir
from concourse.bass_utils import run_bass_kernel_spmd

F32 = mybir.dt.float32
BF16 = mybir.dt.bfloat16
U32 = mybir.dt.uint32
AF = mybir.ActivationFunctionType
ALU = mybir.AluOpType
AX = mybir.AxisListType

D = 1024
DIN = 3584
NE = 16384
EPS = 1e-6
N_CORES = 8
TT = 256
NSUB = TT // 128
GI = 1
NS = 4


class Prog:
    def __init__(self):
        self.ops = []
        self.cap = None
        self.defer = {}

    def op(self, eng, fn, r=(), w=(), dma=None, cost=None, defer=0):
        rec = (eng, fn, tuple(r), tuple(w), dma)
        if defer:
            self.defer[id(rec)] = defer
        if self.cap is not None:
            self.cap.append((rec, 0.3 if cost is None else cost))
        else:
            self.ops.append(rec)

    def emit(self, nc, stack):
        ops = self.ops
        n = len(ops)
        last_w, readers = {}, {}
        deps = [None] * n
        for i, (eng, fn, r, w, dma) in enumerate(ops):
            d = set()
            for k in r:
                if k in last_w:
                    d.add(last_w[k])
            for k in w:
                if k in last_w:
                    d.add(last_w[k])
                d.update(readers.get(k, ()))
            d.discard(i)
            deps[i] = d
            for k in r:
                readers.setdefault(k, []).append(i)
            for k in w:
                last_w[k] = i
                readers[k] = []

        def prod(j):
            return ('dma', ops[j][4]) if ops[j][4] is not None else ('eng', ops[j][0])

        needed = [False] * n
        red = [None] * n
        for i in range(n):
            m = {}
            for j in deps[i]:
                p = prod(j)
                if p == ('eng', 'pe') and ops[i][0] == 'pe' and ops[i][4] is None:
                    continue
                if p not in m or m[p] < j:
                    m[p] = j
            red[i] = m
            for j in m.values():
                needed[j] = True
        cnt, val = {}, [0] * n
        for j in range(n):
            p = prod(j)
            if p[0] == 'dma':
                cnt[p] = cnt.get(p, 0) + 16
                val[j] = cnt[p]
            elif needed[j]:
                cnt[p] = cnt.get(p, 0) + 1
                val[j] = cnt[p]
        sems = {}
        for idx, p in enumerate(sorted(cnt.keys(), key=str)):
            sems[p] = stack.enter_context(nc.semaphore("sm%d" % idx))
        self.n_sems = len(sems)
        block = stack.enter_context(nc.Block())

        def make(engname):
            def body(e):
                waited = {}
                for i, (eng, fn, r, w, dma) in enumerate(ops):
                    if eng != engname:
                        continue
                    for p, j in red[i].items():
                        v = val[j]
                        if waited.get(p, 0) < v:
                            e.wait_ge(sems[p], v)
                            waited[p] = v
                    if fn is None:
                        continue
                    ins = fn(e)
                    if dma is not None:
                        ins.then_inc(sems[prod(i)], 16)
                    elif needed[i]:
                        ins.then_inc(sems[prod(i)], 1)
            return body

        block.tensor(make('pe'))
        block.scalar(make('act'))
        block.vector(make('dve'))
        block.gpsimd(make('pool'))
        block.sync(make('sp'))


def build_nc(n_seq, seq_len, debug=()):
    ntok = n_seq * seq_len
    assert seq_len % TT == 0
    nblk = ntok // TT
    blk_per_seq = seq_len // TT
    nc = bass.Bass("TRN2", target_bir_lowering=False)
    P = Prog()
    st = ExitStack()

    def dram_in(name, shape, dt=F32):
        return nc.dram_tensor(name, list(shape), dt, kind="ExternalInput").ap()

    x_d = dram_in("x", [ntok, D])
    gm_d = dram_in("gm_pk", [128, 8])
    gf_d = dram_in("gf_pk", [128, 8])
    gfin_d = dram_in("gfin_bc", [128, D])
    NWG = 13
    wcat_d = dram_in("wcat", [NWG * 128, 4096])
    cw_d = dram_in("cw", [128, 4, 3])
    cgain_d = dram_in("cgain", [128, 4])
    lbl_d = dram_in("lbl", [128, 4, 2])
    hgain_d = dram_in("hgain", [128, 1])
    kT_d = dram_in("kT", [128, 16, 128])
    NG = 128 // GI
    UT_d = dram_in("UT", [NG * 128, 8 * GI * 128])
    V_d = dram_in("V", [NG * 128, GI * D])
    out_d = nc.dram_tensor("out", [ntok, D], F32, kind="ExternalOutput").ap()
    wcatb_d = nc.dram_tensor("wcat_b", [NWG * 128, 4096], BF16, kind="Internal").ap()
    UTb_d = nc.dram_tensor("UT_b", [NG * 128, 8 * GI * 128], BF16, kind="Internal").ap()
    Vb_d = nc.dram_tensor("V_b", [NG * 128, GI * D], BF16, kind="Internal").ap()

    def sb(name, shape, dt=F32):
        return st.enter_context(nc.sbuf_tensor(name, list(shape), dt))

    PS = [st.enter_context(nc.psum_tensor("ps%d" % i, [128, 512], F32)) for i in range(8)]
    PK = ["ps%d" % i for i in range(8)]

    identF = sb("identF", [128, 128]); identB = sb("identB", [128, 128], BF16)
    iotaF = sb("iotaF", [128, 128]); iotaB = sb("iotaB", [128, 128], BF16)
    causal = sb("causal", [128, 128])
    onesdiv = sb("onesdiv", [128, 128]); grp64 = sb("grp64", [128, 128])
    ones_t = sb("ones_t", [128, 128])
    eps_t = sb("eps_t", [128, 1]); nhalf = sb("nhalf", [128, 1])
    gm_bc = sb("gm_pk_s", [128, 8]); gf_bc = sb("gf_pk_s", [128, 8]); gfin_bc = sb("gfin_bc_s", [128, D])
    cw = sb("cw_s", [128, 4, 3]); cgain = sb("cgain_s", [128, 4]); lbl = sb("lbl_s", [128, 4, 2])
    lb = sb("lb", [128, 4]); oml = sb("oml", [128, 4]); hgain = sb("hgain_s", [128, 1])
    Wst = sb("Wst", [128, 2, 8, 512], BF16)
    kT_sb = sb("kT_sb", [128, 16, 128], BF16)
    BIG = sb("BIG", [128, 32768], BF16)
    WT = BIG[:, 0:128 * TT].rearrange("p (t i) -> p t i", i=128)

    xres = sb("xres", [128, 2, NSUB, D])
    hb = sb("hb", [128, D], BF16)
    hT = sb("hT", [128, 8, 128], BF16)
    stat = sb("stat", [128, 8])
    v_sb = sb("v_sb", [128, 512], BF16)
    cgs = sb("cgs", [128, 4, 128]); hist = sb("hist", [128, 4, 2])
    uext = sb("uext", [128, 4, 130])
    ycv = sb("ycv", [128, 4, 128])
    rs = sb("rs", [128, 512])
    mixT = sb("mixT", [128, 8, 128], BF16)
    qs = sb("qs", [128, 4, 128]); ff = sb("ff", [128, 4, 128]); kh = sb("kh", [128, 4, 128])
    logf = sb("logf", [128, 4, 128]); Gt = sb("Gt", [128, 4, 128]); gs = sb("gs", [128, 4, 128])
    negref = sb("negref", [128, 4]); dec = sb("dec", [128, 4])
    qdT = sb("qdT", [128, 4, 128], BF16); kdT = sb("kdT", [128, 4, 128], BF16)
    qd0T = sb("qd0T", [128, 4, 128], BF16); keT = sb("keT", [128, 4, 128], BF16)
    AmT = sb("AmT", [128, 4, 128], BF16); kend_tm = sb("kend_tm", [128, 4, 128], BF16)
    S = sb("S", [128, 4, 128]); S_bf = sb("S_bf", [128, 4, 128], BF16)
    o2 = sb("o2", [128, 512])
    ytmp = rs[:].rearrange("p (a t) -> p a t", a=4); Ex = o2[:].rearrange("p (a t) -> p a t", a=4)
    h2T = sb("h2T", [128, 2, 8, TT], BF16)
    qT = sb("qT", [128, 16, 128], BF16)
    sc_sb = sb("sc_sb", [128, 16, 128])
    V16 = sb("V16", [128, 16, 16]); IX = sb("IX", [128, 16, 16], U32); IXf = sb("IXf", [128, 16, 16])
    cand = sb("cand", [128, 8, 256])
    CS = sb("CS", [128, 8, 16]); CP = sb("CP", [128, 8, 16], U32)
    csh = sb("csh", [128, 8, 16]); ee = csh; Zs = sb("Zs", [128, 8]); rz = sb("rz", [128, 8])
    Gg = sb("Gg", [128, 8, 16])
    r1u = sb("r1u", [128, 8, 16], U32); r2u = sb("r2u", [128, 8, 16], U32)
    r1f = sb("r1f", [128, 8, 16]); r2f = sb("r2f", [128, 8, 16])
    oh = cand[:].rearrange("p h (a b) -> p h a b", a=16)
    oh2 = sc_sb[:].rearrange("p a b -> p (a b)").rearrange("p (h a b) -> p h a b", h=8, a=16)
    I1f = sb("I1f", [128, 8, 16]); I2f = sb("I2f", [128, 8, 16])
    slots = sb("slots", [128, 3, TT])
    NOH = 4
    P2b = sb("P2b", [128, NOH, 128], BF16); GPb = sb("GPb", [128, NOH, 128], BF16)
    ge = sb("ge", [128, 4, TT], BF16); WA = sb("WA", [128, 4, TT], BF16)
    Ubuf = sb("Ubuf", [128, NS, 8, GI * 128], BF16); Vbuf = sb("Vbuf", [128, NS, GI, D], BF16)

    dbg_outs = {}

    def dump(name, ap, key, shape, dt=F32):
        if name not in debug:
            return
        cntn = sum(1 for k in dbg_outs if k.startswith(name))
        nm = "%s_%d" % (name, cntn)
        dd = nc.dram_tensor("dbg_" + nm, list(shape), dt, kind="ExternalOutput").ap()
        dbg_outs[nm] = True
        P.op('sp', lambda e, dd=dd, ap=ap: e.dma_start(out=dd, in_=ap), r=[key], w=['dbgout_' + nm], dma='dbg')

    P.op('pool', lambda e: e.iota(iotaF[:], pattern=[[1, 128]], base=0, channel_multiplier=0,
                                  allow_small_or_imprecise_dtypes=True), w=['iotaF'])
    P.op('pool', lambda e: e.memset(identF[:], 1.0), w=['identF'])
    P.op('pool', lambda e: e.affine_select(out=identF[:], in_=identF[:], pattern=[[1, 128]],
                                           compare_op=ALU.is_equal, fill=0.0, base=0, channel_multiplier=-1),
         r=['identF'], w=['identF'])
    P.op('pool', lambda e: e.memset(causal[:], 1.0), w=['causal'])
    P.op('pool', lambda e: e.affine_select(out=causal[:], in_=causal[:], pattern=[[1, 128]],
                                           compare_op=ALU.is_ge, fill=0.0, base=0, channel_multiplier=-1),
         r=['causal'], w=['causal'])
    P.op('pool', lambda e: e.memset(onesdiv[:], 1.0 / 128), w=['onesdiv'])
    P.op('pool', lambda e: e.memset(grp64[:], 0.0), w=['grp64'])
    P.op('pool', lambda e: e.memset(grp64[0:64, 0:64], 1.0 / 64), r=['grp64'], w=['grp64'])
    P.op('pool', lambda e: e.memset(grp64[64:128, 64:128], 1.0 / 64), r=['grp64'], w=['grp64'])
    P.op('pool', lambda e: e.memset(ones_t[:], 1.0), w=['ones_t'])
    P.op('pool', lambda e: e.memset(eps_t[:], EPS), w=['eps_t'])
    P.op('pool', lambda e: e.memset(nhalf[:], -0.5), w=['nhalf'])
    P.op('dve', lambda e: e.tensor_copy(out=identB[:], in_=identF[:]), r=['identF'], w=['identB'])
    P.op('dve', lambda e: e.tensor_copy(out=iotaB[:], in_=iotaF[:]), r=['iotaF'], w=['iotaB'])

    for nm, dd, ss_ in (("gm", gm_d, gm_bc), ("gf", gf_d, gf_bc), ("gfin", gfin_d, gfin_bc), ("cw", cw_d, cw),
                        ("cgain", cgain_d, cgain), ("lbl", lbl_d, lbl), ("hgain", hgain_d, hgain)):
        P.op('sp', lambda e, dd=dd, ss_=ss_: e.dma_start(out=ss_[:], in_=dd), w=[nm], dma='par_' + nm)
    P.op('dve', lambda e: e.tensor_tensor(out=stat[:, 0:4], in0=lbl[:, :, 0], in1=lbl[:, :, 1], op=ALU.subtract),
         r=['lbl'], w=['stat'])
    P.op('act', lambda e: e.activation(out=lb[:], in_=stat[:, 0:4], func=AF.Sigmoid), r=['stat'], w=['lb'])
    P.op('dve', lambda e: e.tensor_scalar(out=oml[:], in0=lb[:], scalar1=-0.5, scalar2=0.5, op0=ALU.mult, op1=ALU.add),
         r=['lb'], w=['oml'])
    P.op('dve', lambda e: e.tensor_tensor(out=lb[:], in0=lb[:], in1=oml[:], op=ALU.add), r=['lb', 'oml'], w=['lb'])

    for k in range(NWG):
        P.op('pool', lambda e, k=k: e.dma_start(out=wcatb_d[k * 128:(k + 1) * 128, :], in_=wcat_d[k * 128:(k + 1) * 128, :]),
             w=['wcb%d' % k], dma='cast_w')
    P.op('pool', lambda e: e.dma_start(out=kT_sb[:], in_=kT_d), w=['kT_sb'], dma='cast_kT')
    NCU = 32
    RPC = NG * 128 // NCU
    for c in range(NCU):
        P.op('pool', lambda e, c=c: e.dma_start(out=UTb_d[c * RPC:(c + 1) * RPC, :], in_=UT_d[c * RPC:(c + 1) * RPC, :]),
             w=['UTb%d' % c], dma='cast_U')
        P.op('pool', lambda e, c=c: e.dma_start(out=Vb_d[c * RPC:(c + 1) * RPC, :], in_=V_d[c * RPC:(c + 1) * RPC, :]),
             w=['Vb%d' % c], dma='cast_V')
    ALL_UTB = ['UTb%d' % c for c in range(NCU)]
    ALL_VB = ['Vb%d' % c for c in range(NCU)]

    SC = 128.0 ** -0.5

    def rmsnorm_to_bf16(src_ap, src_keys, gain_bc, gain_key):
        P.op('dve', lambda e: e.tensor_tensor(out=hb[:], in0=src_ap, in1=src_ap, op=ALU.mult), r=src_keys, w=['hb'], cost=1.2)
        P.op('dve', lambda e: e.reduce_sum(out=stat[:, 4:5], in_=hb[:], axis=AX.X), r=['hb'], w=['stat'], cost=1.2)
        P.op('dve', lambda e: e.tensor_scalar(out=stat[:, 5:6], in0=stat[:, 4:5], scalar1=1.0 / D, scalar2=EPS, op0=ALU.mult, op1=ALU.add),
             r=['stat'], w=['stat'], cost=0.15)
        P.op('pool', lambda e: e.tensor_tensor(out=stat[:, 6:7], in0=stat[:, 5:6], in1=nhalf[:], op=ALU.pow),
             r=['stat', 'nhalf'], w=['stat'], cost=0.5)

    BX, BY = 6, 7
    wld = [0]

    pending = {}

    def issue_w(grp):
        slot = wld[0] % 2
        wld[0] += 1
        P.op('pool', lambda e: e.dma_start(out=Wst[:, slot, :, :].rearrange("p k n -> p (k n)"), in_=wcatb_d[grp * 128:(grp + 1) * 128, :]),
             r=['wcb%d' % grp], w=['Wst%d' % slot], dma='W%d' % slot, cost=0.0)
        pending[grp] = slot

    def load_w(grp):
        if grp not in pending:
            issue_w(grp)
        slot = pending.pop(grp)
        issue_w((grp + 1) % NWG)
        return slot

    def pre(b, sub):
        cur = b % 2
        t0 = b * TT + sub * 128
        first = (b % blk_per_seq == 0) and sub == 0
        xr = xres[:, cur, sub, :]
        xk = 'xres%d_%d' % (cur, sub)
        hk = 'h2T%d' % cur
        tsl = slice(sub * 128, (sub + 1) * 128)
        P.op('pool', lambda e: e.dma_start(out=xr, in_=x_d[t0:t0 + 128, :]), w=[xk], dma='xload%d_%d' % (cur, sub), cost=0.0)
        if first:
            P.op('pool', lambda e: e.memset(S[:], 0.0), w=['S'], cost=0.0)
            P.op('pool', lambda e: e.memset(S_bf[:], 0.0), w=['S_bf'], cost=0.0)
            P.op('pool', lambda e: e.memset(uext[:, :, 0:2], 0.0), w=['uext'], cost=0.0)
        rmsnorm_to_bf16(xr, [xk], None, None)
        P.op('dve', lambda e: e.tensor_scalar(out=hb[:], in0=xr, scalar1=stat[:, 6:7], scalar2=None, op0=ALU.mult),
             r=[xk, 'stat'], w=['hb'], cost=1.2)
        psx = PS[BX][:].bitcast(BF16)
        psy = PS[BY][:].bitcast(BF16)
        for k in range(8):
            P.op('pe', lambda e, k=k: e.transpose(psx[:, k * 128:(k + 1) * 128], hb[:, k * 128:(k + 1) * 128], identB[:]),
                 r=['hb', 'identB'], w=[PK[BX]], cost=0.12, defer=(4 if k == 0 else 0))
        for k in range(8):
            P.op('act', lambda e, k=k: e.activation(out=hT[:, k, :], in_=psx[:, k * 128:(k + 1) * 128], func=AF.Copy, scale=gm_bc[:, k:k + 1]),
                 r=[PK[BX], 'gm'], w=['hT'], cost=0.3)
        ws = load_w(0)
        for k in range(8):
            P.op('pe', lambda e, k=k: e.matmul(PS[BY][:], hT[:, k, :], Wst[:, ws, k, :], start=(k == 0), stop=(k == 7)),
                 r=['hT', 'Wst%d' % ws], w=[PK[BY]], cost=0.27)
        P.op('act', lambda e: e.activation(out=v_sb[:], in_=PS[BY][:], func=AF.Copy), r=[PK[BY]], w=['v_sb'], cost=0.6)

        def fm_group(grp, bank):
            wsl = load_w(grp)
            for j in range(4):
                for k in range(8):
                    P.op('pe', lambda e, k=k, j=j: e.matmul(PS[bank][:, j * 128:(j + 1) * 128], Wst[:, wsl, k, j * 128:(j + 1) * 128], hT[:, k, :],
                                                            start=(k == 0), stop=(k == 7)),
                         r=['hT', 'Wst%d' % wsl], w=[PK[bank]], cost=0.12)
        qs2, ff2, gs2 = [t[:].rearrange("p a t -> p (a t)") for t in (qs, ff, gs)]
        fm_group(1, BX)
        P.op('act', lambda e: e.activation(out=cgs[:].rearrange("p a t -> p (a t)"), in_=PS[BX][:], func=AF.Copy), r=[PK[BX]], w=['cgs'], cost=0.6)
        fm_group(2, BY)
        P.op('dve', lambda e: e.tensor_tensor(out=uext[:, :, 2:130], in0=PS[BY][:].rearrange("p (a t) -> p a t", a=4), in1=cgs[:], op=ALU.mult),
             r=[PK[BY], 'cgs'], w=['uext'], cost=0.7)
        for c in range(4):
            P.op('dve', lambda e, c=c: e.tensor_scalar(out=ycv[:, c, :], in0=uext[:, c, 0:128], scalar1=cw[:, c, 0:1],
                                                       scalar2=None, op0=ALU.mult), r=['uext', 'cw'], w=['ycv%d' % c], cost=0.25)
        for c in range(4):
            P.op('dve', lambda e, c=c: e.scalar_tensor_tensor(out=ytmp[:, c, :], in0=uext[:, c, 1:129], scalar=cw[:, c, 1:2],
                                                              in1=ycv[:, c, :], op0=ALU.mult, op1=ALU.add),
                 r=['uext', 'cw', 'ycv%d' % c], w=['yt%d' % c] + (['rs'] if c == 0 else []), cost=0.3)
        for c in range(4):
            P.op('dve', lambda e, c=c: e.scalar_tensor_tensor(out=ycv[:, c, :], in0=uext[:, c, 2:130], scalar=cw[:, c, 2:3],
                                                              in1=ytmp[:, c, :], op0=ALU.mult, op1=ALU.add),
                 r=['uext', 'cw', 'yt%d' % c], w=['ycv%d' % c], cost=0.3)
        YCV = ['ycv%d' % c for c in range(4)]
        YT = ['yt%d' % c for c in range(4)]
        P.op('dve', lambda e: e.tensor_copy(out=hist[:], in_=uext[:, :, 128:130]), r=['uext'], w=['hist'], cost=0.1)
        P.op('dve', lambda e: e.tensor_copy(out=uext[:, :, 0:2], in_=hist[:]), r=['hist'], w=['uext'], cost=0.1)
        fm_group(3, BX)
        ycv2 = ycv[:].rearrange("p a t -> p (a t)")
        P.op('dve', lambda e: e.tensor_tensor(out=ycv2, in0=PS[BX][:], in1=ycv2, op=ALU.mult), r=[PK[BX]] + YCV, w=['ycv'] + YCV, cost=0.7)
        P.op('dve', lambda e: e.tensor_tensor(out=o2[:], in0=ycv2, in1=ycv2, op=ALU.mult), r=['ycv'] + YCV, w=['o2'], cost=0.7)
        P.op('pe', lambda e: e.matmul(PS[BY][:], grp64[:], o2[:], start=True, stop=True), r=['grp64', 'o2'], w=[PK[BY]], cost=1.0, defer=2)
        P.op('act', lambda e: e.activation(out=rs[:], in_=PS[BY][:], func=AF.Sqrt, bias=eps_t[:], scale=1.0),
             r=[PK[BY], 'eps_t'], w=['rs'] + YT, cost=0.6)
        P.op('dve', lambda e: e.reciprocal(out=rs[:], in_=rs[:]), r=['rs'], w=['rs'], cost=1.2)
        for c in range(4):
            P.op('dve', lambda e, c=c: e.scalar_tensor_tensor(out=mixT[:, c, :], in0=ycv[:, c, :], scalar=cgain[:, c:c + 1],
                                                              in1=rs[:, c * 128:(c + 1) * 128], op0=ALU.mult, op1=ALU.mult),
                 r=['ycv', 'ycv%d' % c, 'cgain', 'rs'], w=['mixT%d' % c], cost=0.3)
        fm_group(4, BX)
        P.op('act', lambda e: e.activation(out=qs2, in_=PS[BX][:], func=AF.Tanh, scale=0.5), r=[PK[BX]], w=['qs'], cost=0.6)
        P.op('dve', lambda e: e.scalar_tensor_tensor(out=qs2, in0=qs2, scalar=1.0, in1=PS[BX][:], op0=ALU.add, op1=ALU.mult),
             r=['qs', PK[BX]], w=['qs'], cost=0.7)
        fm_group(5, BY)
        P.op('act', lambda e: e.activation(out=ff2, in_=PS[BY][:], func=AF.Tanh, scale=0.5), r=[PK[BY]], w=['ff'], cost=0.6)
        fm_group(6, BX)
        P.op('act', lambda e: e.activation(out=gs2, in_=PS[BX][:], func=AF.Tanh, scale=0.5), r=[PK[BX]], w=['gs'], cost=0.6)
        P.op('dve', lambda e: e.scalar_tensor_tensor(out=gs2, in0=gs2, scalar=1.0, in1=PS[BX][:], op0=ALU.add, op1=ALU.mult),
             r=['gs', PK[BX]], w=['gs'], cost=0.7)
        for h in range(4):
            P.op('dve', lambda e, h=h: e.tensor_scalar(out=ff[:, h, :], in0=ff[:, h, :], scalar1=oml[:, h:h + 1],
                                                       scalar2=lb[:, h:h + 1], op0=ALU.mult, op1=ALU.add),
                 r=['ff', 'oml', 'lb'], w=['ff'], cost=0.3)
        kh2, lg2, Gt2 = [t[:].rearrange("p a t -> p (a t)") for t in (kh, logf, Gt)]
        Ex2 = o2[:]
        P.op('dve', lambda e: e.tensor_scalar(out=kh2, in0=ff2, scalar1=-1.0, scalar2=1.0, op0=ALU.mult, op1=ALU.add),
             r=['ff'], w=['kh'], cost=0.5)
        P.op('act', lambda e: e.activation(out=lg2, in_=ff2, func=AF.Ln), r=['ff'], w=['logf'], cost=0.6, defer=2)
        for h in range(4):
            P.op('dve', lambda e, h=h: e.tensor_tensor_scan(out=Gt[:, h, :], data0=ones_t[:], data1=logf[:, h, :], initial=0.0,
                                                            op0=ALU.mult, op1=ALU.add), r=['ones_t', 'logf'], w=['Gt%d' % h], cost=0.35)
        GT = ['Gt%d' % h for h in range(4)]
        P.op('dve', lambda e: e.tensor_scalar(out=negref[:], in0=Gt[:, :, 63], scalar1=-1.0, scalar2=None, op0=ALU.mult),
             r=GT, w=['negref'], cost=0.15)
        P.op('act', lambda e: e.activation(out=dec[:], in_=Gt[:, :, 127], func=AF.Exp), r=GT, w=['dec'], cost=0.25, defer=2)
        for h in range(4):
            P.op('act', lambda e, h=h: e.activation(out=qdT[:, h, :], in_=Gt[:, h, :], func=AF.Exp, bias=negref[:, h:h + 1], scale=1.0),
                 r=GT + ['negref'], w=['qdT'], cost=0.3)
        for h in range(4):
            P.op('act', lambda e, h=h: e.activation(out=kdT[:, h, :], in_=Gt[:, h, :], func=AF.Exp, bias=Gt[:, h, 63:64], scale=-1.0),
                 r=GT, w=['kdT'], cost=0.3)
        P.op('act', lambda e: e.activation(out=qd0T[:].rearrange("p a t -> p (a t)"), in_=Gt2, func=AF.Exp), r=GT, w=['qd0T'], cost=0.6)
        for h in range(4):
            P.op('act', lambda e, h=h: e.activation(out=keT[:, h, :], in_=Gt[:, h, :], func=AF.Exp, bias=Gt[:, h, 127:128], scale=-1.0),
                 r=GT, w=['keT'], cost=0.3)
        SCH = 0.5 * SC
        qdT2, kdT2, qd0T2, keT2 = [t[:].rearrange("p a t -> p (a t)") for t in (qdT, kdT, qd0T, keT)]
        P.op('dve', lambda e: e.scalar_tensor_tensor(out=qdT2, in0=qs2, scalar=SCH, in1=qdT2, op0=ALU.mult, op1=ALU.mult),
             r=['qs', 'qdT'], w=['qdT'], cost=0.7)
        P.op('dve', lambda e: e.tensor_tensor(out=kdT2, in0=kh2, in1=kdT2, op=ALU.mult), r=['kh', 'kdT'], w=['kdT'], cost=0.7)
        P.op('dve', lambda e: e.scalar_tensor_tensor(out=qd0T2, in0=qs2, scalar=SCH, in1=qd0T2, op0=ALU.mult, op1=ALU.mult),
             r=['qs', 'qd0T'], w=['qd0T'], cost=0.7)
        P.op('dve', lambda e: e.tensor_tensor(out=keT2, in0=kh2, in1=keT2, op=ALU.mult), r=['kh', 'keT'], w=['keT'], cost=0.7)
        for h in range(4):
            P.op('pe', lambda e, h=h: e.matmul(PS[BY][:, h * 128:(h + 1) * 128], kdT[:, h, :], qdT[:, h, :], start=True, stop=True),
                 r=['kdT', 'qdT'], w=[PK[BY]], cost=0.12, defer=(2 if h == 0 else 0))
        P.op('dve', lambda e: e.tensor_tensor(out=AmT[:], in0=PS[BY][:].rearrange("p (a t) -> p a t", a=4),
                                              in1=causal[:].unsqueeze(1).broadcast_to([128, 4, 128]), op=ALU.mult),
             r=[PK[BY], 'causal'], w=['AmT'], cost=0.7)
        for h in range(4):
            P.op('pe', lambda e, h=h: e.transpose(psx[:, h * 128:(h + 1) * 128], keT[:, h, :], identB[:]),
                 r=['keT', 'identB'], w=[PK[BX]], cost=0.12, defer=(2 if h == 0 else 0))
        P.op('act', lambda e: e.activation(out=kend_tm[:].rearrange("p a t -> p (a t)"), in_=psx[:, 0:512], func=AF.Copy),
             r=[PK[BX]], w=['kend_tm'], cost=0.6)
        for h in range(4):
            P.op('pe', lambda e, h=h: e.matmul(PS[BY][:, h * 128:(h + 1) * 128], v_sb[:, h * 128:(h + 1) * 128], AmT[:, h, :],
                                               start=True, stop=False), r=['v_sb', 'AmT'], w=[PK[BY]], cost=0.12, defer=(2 if h == 0 else 0))
            P.op('pe', lambda e, h=h: e.matmul(PS[BY][:, h * 128:(h + 1) * 128], S_bf[:, h, :], qd0T[:, h, :],
                                               start=False, stop=True), r=['S_bf', 'qd0T'], w=[PK[BY]], cost=0.12)
        for h in range(4):
            P.op('pe', lambda e, h=h: e.matmul(PS[BX][:, h * 128:(h + 1) * 128], kend_tm[:, h, :], v_sb[:, h * 128:(h + 1) * 128],
                                               start=True, stop=True), r=['kend_tm', 'v_sb'], w=[PK[BX]], cost=0.12)
        for h in range(4):
            P.op('dve', lambda e, h=h: e.scalar_tensor_tensor(out=S[:, h, :], in0=S[:, h, :], scalar=dec[:, h:h + 1],
                                                              in1=PS[BX][:, h * 128:(h + 1) * 128], op0=ALU.mult, op1=ALU.add),
                 r=['S', 'dec', PK[BX]], w=['S'], cost=0.3)
        P.op('dve', lambda e: e.tensor_copy(out=S_bf[:], in_=S[:]), r=['S'], w=['S_bf'], cost=0.5)
        P.op('act', lambda e: e.activation(out=o2[:], in_=PS[BY][:], func=AF.Square), r=[PK[BY]], w=['o2'], cost=0.6)
        P.op('pe', lambda e: e.matmul(PS[BX][:], onesdiv[:], o2[:], start=True, stop=True), r=['onesdiv', 'o2'], w=[PK[BX]], cost=1.0)
        P.op('act', lambda e: e.activation(out=rs[:], in_=PS[BX][:], func=AF.Sqrt, bias=eps_t[:], scale=1.0),
             r=[PK[BX], 'eps_t'], w=['rs'], cost=0.6)
        P.op('dve', lambda e: e.reciprocal(out=rs[:], in_=rs[:]), r=['rs'], w=['rs'], cost=1.2)
        P.op('dve', lambda e: e.scalar_tensor_tensor(out=rs[:], in0=PS[BY][:], scalar=hgain[:, 0:1], in1=rs[:],
                                                     op0=ALU.mult, op1=ALU.mult), r=[PK[BY], 'hgain', 'rs'], w=['rs'], cost=0.7)
        P.op('dve', lambda e: e.scalar_tensor_tensor(out=mixT[:, 4:8, :].rearrange("p a t -> p (a t)"), in0=rs[:], scalar=0.5, in1=gs2,
                                                     op0=ALU.mult, op1=ALU.mult), r=['rs', 'gs'], w=['mixT4'], cost=0.7)
        MIXK = ['mixT%d' % c for c in range(5)]
        dump('mixT', mixT[:], 'mixT4', [128, 8, 128], BF16)
        for half, bank in ((0, BY), (1, BX)):
            wsl = load_w(7 + half)
            for k in range(8):
                P.op('pe', lambda e, k=k, wsl=wsl, bank=bank: e.matmul(PS[bank][:], mixT[:, k, :], Wst[:, wsl, k, :], start=(k == 0), stop=(k == 7)),
                     r=MIXK + ['Wst%d' % wsl], w=[PK[bank]], cost=0.27, defer=(2 if (half == 0 and k == 0) else 0))
            P.op('dve', lambda e, half=half, bank=bank: e.tensor_tensor(out=xr[:, half * 512:(half + 1) * 512], in0=PS[bank][:],
                                                                        in1=xr[:, half * 512:(half + 1) * 512], op=ALU.add),
                 r=[PK[bank], xk], w=[xk], cost=0.7)
        dump('x2', xr, xk, [128, D])
        rmsnorm_to_bf16(xr, [xk], None, None)
        P.op('dve', lambda e: e.tensor_scalar(out=hb[:], in0=xr, scalar1=stat[:, 6:7], scalar2=None, op0=ALU.mult),
             r=[xk, 'stat'], w=['hb'], cost=1.2)
        for k in range(8):
            P.op('pe', lambda e, k=k: e.transpose(psy[:, k * 128:(k + 1) * 128], hb[:, k * 128:(k + 1) * 128], identB[:]),
                 r=['hb', 'identB'], w=[PK[BY]], cost=0.12, defer=(3 if k == 0 else 0))
        for k in range(8):
            P.op('act', lambda e, k=k: e.activation(out=h2T[:, cur, k, tsl], in_=psy[:, k * 128:(k + 1) * 128],
                                                    func=AF.Copy, scale=gf_bc[:, k:k + 1]),
                 r=[PK[BY], 'gf'], w=[hk], cost=0.3)
        marks.append(len(P.cap) if P.cap is not None else -1)
        for c4 in range(4):
            bank = BX if c4 % 2 == 0 else BY
            wsl = load_w(9 + c4)
            for j in range(4):
                for k in range(8):
                    P.op('pe', lambda e, k=k, j=j, wsl=wsl, bank=bank: e.matmul(PS[bank][:, j * 128:(j + 1) * 128],
                                                                              Wst[:, wsl, k, j * 128:(j + 1) * 128], h2T[:, cur, k, tsl],
                                                                              start=(k == 0), stop=(k == 7)),
                         r=[hk, 'Wst%d' % wsl], w=[PK[bank]], cost=0.12)
            P.op('act', lambda e, c4=c4, bank=bank: e.activation(out=qT[:, 4 * c4:4 * c4 + 4, :].rearrange("p a t -> p (a t)"),
                                                                 in_=PS[bank][:], func=AF.Copy), r=[PK[bank]], w=['qT%d' % c4], cost=0.6)
        for c4 in range(4):
            bank = BX if c4 % 2 == 0 else BY
            for j in range(4):
                jj = 4 * c4 + j
                P.op('pe', lambda e, j=j, jj=jj, bank=bank: e.matmul(PS[bank][:, j * 128:(j + 1) * 128], qT[:, jj, :], kT_sb[:, jj, :],
                                                                     start=True, stop=True), r=['qT%d' % c4, 'kT_sb'], w=[PK[bank]], cost=0.12)
            P.op('act', lambda e, c4=c4, bank=bank: e.activation(out=sc_sb[:, 4 * c4:4 * c4 + 4, :].rearrange("p a t -> p (a t)"),
                                                                 in_=PS[bank][:], func=AF.Copy), r=[PK[bank]], w=['sc_sb'], cost=0.6)
        dump('sc', sc_sb[:], 'sc_sb', [128, 16, 128])
        marks.append(len(P.cap) if P.cap is not None else -1)
        scr1 = cand[:].rearrange("p h n -> p (h n)").rearrange("p (j n) -> p j n", j=16)
        tagsA = ['a%d' % j for j in range(16)]
        top16_batch([(sc_sb[:, j, :], V16[:, j, :], IX[:, j, :], scr1[:, j, :], tagsA[j]) for j in range(16)], 'sc_sb', ['cand'])
        KV16 = ['tv' + t for t in tagsA]; KIX = ['ti' + t for t in tagsA]; KS1 = ['ts' + t for t in tagsA]
        P.op('dve', lambda e: e.tensor_copy(out=IXf[:], in_=IX[:]), r=KIX, w=['IXf'])
        v1 = V16[:, 0::2, :].unsqueeze(3).broadcast_to([128, 8, 16, 16])
        v2 = V16[:, 1::2, :].unsqueeze(2).broadcast_to([128, 8, 16, 16])
        P.op('dve', lambda e: e.tensor_tensor(out=cand[:].rearrange("p h (a b) -> p h a b", a=16), in0=v1, in1=v2, op=ALU.add),
             r=KV16, w=['cand'] + KS1, cost=2.3)
        scr2 = sc_sb[:].rearrange("p j n -> p (j n)").rearrange("p (h n) -> p h n", h=8)
        tagsB = ['b%d' % h for h in range(8)]
        top16_batch([(cand[:, h, :], CS[:, h, :], CP[:, h, :], scr2[:, h, :], tagsB[h]) for h in range(8)], 'cand', ['sc_sb'])
        KCS = ['tv' + t for t in tagsB]; KCP = ['ti' + t for t in tagsB]; KS2 = ['ts' + t for t in tagsB]
        P.op('dve', lambda e: e.tensor_tensor(out=csh[:], in0=CS[:], in1=CS[:, :, 0:1].broadcast_to([128, 8, 16]), op=ALU.subtract),
             r=KCS, w=['csh'])
        P.op('act', lambda e: e.activation(out=ee[:], in_=csh[:], func=AF.Exp), r=['csh'], w=['csh'], defer=3)
        P.op('dve', lambda e: e.reduce_sum(out=Zs[:], in_=ee[:], axis=AX.X), r=['csh'], w=['Zs'])
        P.op('dve', lambda e: e.reciprocal(out=rz[:], in_=Zs[:]), r=['Zs'], w=['rz'])
        P.op('dve', lambda e: e.tensor_tensor(out=Gg[:], in0=ee[:], in1=rz[:].unsqueeze(2).broadcast_to([128, 8, 16]), op=ALU.mult),
             r=['csh', 'rz'], w=['Gg'])
        P.op('dve', lambda e: e.tensor_single_scalar(out=r1u[:], in_=CP[:], scalar=4, op=ALU.logical_shift_right), r=KCP, w=['r1u'])
        P.op('dve', lambda e: e.tensor_single_scalar(out=r2u[:], in_=CP[:], scalar=15, op=ALU.bitwise_and), r=KCP, w=['r2u'])
        P.op('dve', lambda e: e.tensor_copy(out=r1f[:], in_=r1u[:]), r=['r1u'], w=['r1f'])
        P.op('dve', lambda e: e.tensor_copy(out=r2f[:], in_=r2u[:]), r=['r2u'], w=['r2f'])
        io16 = iotaF[:, 0:16].unsqueeze(1).unsqueeze(1).broadcast_to([128, 8, 16, 16])
        for (rf, rk, par, If, Ik) in ((r1f, 'r1f', 0, I1f, 'I1f'), (r2f, 'r2f', 1, I2f, 'I2f')):
            P.op('dve', lambda e, rf=rf: e.tensor_tensor(out=oh, in0=rf[:].unsqueeze(3).broadcast_to([128, 8, 16, 16]), in1=io16,
                                                         op=ALU.is_equal), r=[rk, 'iotaF'], w=['cand'], cost=2.3)
            P.op('dve', lambda e, par=par: e.tensor_tensor(out=oh2, in0=oh,
                                                           in1=IXf[:, par::2, :].unsqueeze(2).broadcast_to([128, 8, 16, 16]),
                                                           op=ALU.mult), r=['cand', 'IXf'], w=['sc_sb'] + KS2, cost=2.3)
            P.op('dve', lambda e, If=If: e.reduce_sum(out=If[:], in_=oh2, axis=AX.X), r=['sc_sb'], w=[Ik], cost=2.3)
        dump('I1f', I1f[:], 'I1f', [128, 8, 16]); dump('I2f', I2f[:], 'I2f', [128, 8, 16]); dump('Gg', Gg[:], 'Gg', [128, 8, 16])
        for i, (src, sk) in enumerate(((I1f, 'I1f'), (I2f, 'I2f'), (Gg, 'Gg'))):
            P.op('pe', lambda e, i=i, src=src: e.transpose(PS[BX][:, i * 128:(i + 1) * 128], src[:].rearrange("p h k -> p (h k)"), identF[:]),
                 r=[sk, 'identF'], w=[PK[BX]], defer=(2 if i == 0 else 0))
        P.op('act', lambda e: e.activation(out=slots[:, :, tsl], in_=PS[BX][:, 0:384].rearrange("p (a t) -> p a t", a=3), func=AF.Copy),
             r=[PK[BX]], w=['slots'], cost=0.6)


    def top16_batch(lists, src_key, first_extra_w):
        for (src, va, ix, tmp, tag) in lists:
            P.op('dve', lambda e, src=src, va=va: e.max(out=va[:, 0:8], in_=src), r=[src_key], w=['tv' + tag])
        for (src, va, ix, tmp, tag) in lists:
            P.op('dve', lambda e, src=src, va=va, ix=ix: e.max_index(out=ix[:, 0:8], in_max=va[:, 0:8], in_values=src),
                 r=[src_key, 'tv' + tag], w=['ti' + tag])
        for n, (src, va, ix, tmp, tag) in enumerate(lists):
            P.op('dve', lambda e, src=src, va=va, tmp=tmp: e.match_replace(out=tmp, in_to_replace=va[:, 0:8], in_values=src, imm_value=-1e30),
                 r=[src_key, 'tv' + tag], w=['ts' + tag] + (first_extra_w if n == 0 else []))
        for (src, va, ix, tmp, tag) in lists:
            P.op('dve', lambda e, va=va, tmp=tmp: e.max(out=va[:, 8:16], in_=tmp), r=['ts' + tag], w=['tv' + tag])
        for (src, va, ix, tmp, tag) in lists:
            P.op('dve', lambda e, src=src, va=va, ix=ix: e.max_index(out=ix[:, 8:16], in_max=va[:, 8:16], in_values=src),
                 r=[src_key, 'tv' + tag], w=['ti' + tag])

    ldc = [0]

    def scatter(b):
        for t in range(TT):
            sl = t % NOH
            P.op('dve', lambda e, t=t, sl=sl: e.tensor_scalar(out=P2b[:, sl, :], in0=iotaB[:], scalar1=slots[:, 1, t:t + 1], scalar2=None,
                                                              op0=ALU.is_equal), r=['slots', 'iotaB'], w=['P2b%d' % sl])
            P.op('dve', lambda e, t=t, sl=sl: e.tensor_scalar(out=GPb[:, sl, :], in0=iotaB[:], scalar1=slots[:, 0, t:t + 1],
                                                              scalar2=slots[:, 2, t:t + 1], op0=ALU.is_equal, op1=ALU.mult),
                 r=['slots', 'iotaB'], w=['GPb%d' % sl])
            bank = 4 + (t // 4) % 2
            P.op('pe', lambda e, t=t, sl=sl, bank=bank: e.matmul(PS[bank][:, (t % 4) * 128:(t % 4 + 1) * 128], P2b[:, sl, :], GPb[:, sl, :],
                                                                 start=True, stop=True),
                 r=['P2b%d' % sl, 'GPb%d' % sl], w=[PK[bank]])
            if t % 4 == 3:
                tb = t - 3
                P.op('act', lambda e, tb=tb, bank=bank: e.activation(out=WT[:, tb:tb + 4, :].rearrange("p t i -> p (t i)"),
                                                                     in_=PS[bank][:], func=AF.Copy),
                     r=[PK[bank]], w=['BIG'])

    def dense(b, pre_ops, prev_fin=None):
        cur = b % 2
        hk = 'h2T%d' % cur
        ldcount = ldc[0]
        LAG = 3
        total = sum(c for _, c in pre_ops)
        state = {'i': 0, 'cum': 0.0}
        NSPREAD = 118

        lastw = {}
        prod_np = [-1] * len(pre_ops)
        prod_any = {}
        estep = [0] * len(pre_ops)
        for idx, (rec, c) in enumerate(pre_ops):
            eng, fn, r, w, dma = rec
            if id(rec) in P.defer:
                prod_any[idx] = max([lastw.get(k, -1) for k in r] + [-1])
            if eng == 'pe':
                best = -1
                for k in r:
                    j = lastw.get(k, -1)
                    if j >= 0 and pre_ops[j][0][0] != 'pe':
                        best = max(best, j)
                prod_np[idx] = best
            for k in w:
                lastw[k] = idx

        def emit_pre(step):
            tgt = total * min(1.0, (step + 1) / float(NSPREAD))
            last = step >= 127 + LAG
            start_i = state['i']
            while state['i'] < len(pre_ops) and (state['cum'] < tgt or last):
                rec, c = pre_ops[state['i']]
                if not last and rec[0] == 'pe' and prod_np[state['i']] >= start_i:
                    break
                if not last and state['i'] in prod_any:
                    pj = prod_any[state['i']]
                    if pj >= 0 and step < estep[pj] + P.defer[id(rec)]:
                        break
                P.ops.append(rec)
                estep[state['i']] = step
                state['cum'] += c
                state['i'] += 1

        def load_U(g):
            slot = (ldcount + g) % NS
            P.op('sp', lambda e: e.dma_start(out=Ubuf[:, slot, :, :].rearrange("p k n -> p (k n)"), in_=UTb_d[g * 128:(g + 1) * 128, :]),
                 r=ALL_UTB, w=['Ubuf%d' % slot], dma='U%d' % slot)

        def load_V(g):
            slot = (ldcount + g) % NS
            P.op('sp', lambda e: e.dma_start(out=Vbuf[:, slot, :, :].rearrange("p c d -> p (c d)"), in_=Vb_d[g * 128:(g + 1) * 128, :]),
                 r=ALL_VB, w=['Vbuf%d' % slot], dma='V%d' % slot)

        def u_step(i1):
            g, j = divmod(i1, GI)
            slot = (ldcount + g) % NS
            ab = 4 + i1 % 2
            aps = PS[ab][:, 0:TT]
            for k in range(8):
                P.op('pe', lambda e, k=k: e.matmul(aps, Ubuf[:, slot, k, j * 128:(j + 1) * 128], h2T[:, cur, k, :], start=(k == 0), stop=(k == 7)),
                     r=['Ubuf%d' % slot, hk], w=[PK[ab]])
            gsl = i1 % 4
            P.op('act', lambda e: e.activation(out=ge[:, gsl, :], in_=aps, func=AF.Gelu), r=[PK[ab]], w=['ge%d' % gsl])
            ws = i1 % 4
            P.op('pool', lambda e: e.tensor_tensor(out=WA[:, ws, :], in0=ge[:, gsl, :], in1=WT[:, :, i1], op=ALU.mult),
                 r=['ge%d' % gsl, 'BIG'], w=['WA%d' % ws])

        def v_step(i1):
            g, j = divmod(i1, GI)
            slot = (ldcount + g) % NS
            ws = i1 % 4
            for sub in range(NSUB):
                for half in range(2):
                    yb = sub * 2 + half
                    P.op('pe', lambda e, sub=sub, half=half, yb=yb: e.matmul(PS[yb][:], WA[:, ws, sub * 128:(sub + 1) * 128],
                                                                             Vbuf[:, slot, j, half * 512:(half + 1) * 512],
                                                                             start=(i1 == 0), stop=(i1 == 127)),
                         r=['WA%d' % ws, 'Vbuf%d' % slot], w=[PK[yb]])

        NGRP = 128 // GI
        for g0 in range(NS - 1):
            load_U(g0)
        for g0 in range(NS):
            load_V(g0)
        if prev_fin is not None:
            prev_fin()
        for step in range(128 + LAG):
            if step < 128:
                if step % GI == 0 and step // GI + NS - 1 < NGRP:
                    load_U(step // GI + NS - 1)
                u_step(step)
            if step >= LAG:
                iv = step - LAG
                v_step(iv)
                if (iv + 1) % GI == 0 and iv // GI + NS < NGRP:
                    load_V(iv // GI + NS)
            emit_pre(step)
        ldc[0] = ldcount + 128 // GI
        for sub in range(NSUB):
            xk = 'xres%d_%d' % (cur, sub)
            xr = xres[:, cur, sub, :]
            for half in range(2):
                P.op('dve', lambda e, sub=sub, half=half, xr=xr: e.tensor_tensor(out=xr[:, half * 512:(half + 1) * 512],
                                                                                 in0=PS[sub * 2 + half][:],
                                                                                 in1=xr[:, half * 512:(half + 1) * 512], op=ALU.add),
                     r=[PK[sub * 2 + half], xk], w=[xk])

        def fin():
            for sub in range(NSUB):
                t0 = b * TT + sub * 128
                xk = 'xres%d_%d' % (cur, sub)
                xr = xres[:, cur, sub, :]
                rmsnorm_to_bf16(xr, [xk], None, None)
                P.op('dve', lambda e, xr=xr: e.scalar_tensor_tensor(out=xr, in0=xr, scalar=stat[:, 6:7], in1=gfin_bc[:],
                                                                    op0=ALU.mult, op1=ALU.mult), r=[xk, 'stat', 'gfin'], w=[xk])
                P.op('sp', lambda e, t0=t0, xr=xr: e.dma_start(out=out_d[t0:t0 + 128, :], in_=xr), r=[xk], w=['outdram'], dma='ost')
        return fin

    marks = []

    def merge_by_cost(la, lb_):
        ta = sum(c for _, c in la) or 1.0
        tb = sum(c for _, c in lb_) or 1.0
        out, ia, ib, ca, cb = [], 0, 0, 0.0, 0.0
        while ia < len(la) or ib < len(lb_):
            if ib >= len(lb_) or (ia < len(la) and ca / ta <= cb / tb):
                out.append(la[ia]); ca += la[ia][1]; ia += 1
            else:
                out.append(lb_[ib]); cb += lb_[ib][1]; ib += 1
        return out

    def capture_pre(b):
        assert NSUB == 2
        P.cap = []
        del marks[:]
        pre(b, 0)
        s1 = len(P.cap)
        pre(b, 1)
        ops_, P.cap = P.cap, None
        h0, t0, h1, t1 = marks
        A, T0, B, H1, T1 = ops_[:t0], ops_[t0:s1], ops_[s1:h1], ops_[h1:t1], ops_[t1:]
        return A + merge_by_cost(T0, B) + H1 + T1

    for rec, _ in capture_pre(0):
        P.ops.append(rec)
    fin_prev = None
    for b in range(nblk):
        nxt = capture_pre(b + 1) if b + 1 < nblk else []
        scatter(b)
        fin_prev = dense(b, nxt, fin_prev)
    fin_prev()
    fin = ['outdram'] + ['dbgout_' + k for k in dbg_outs]
    P.op('sp', None, r=fin)
    P.emit(nc, st)
    nc._keep = st
    return nc, list(dbg_outs.keys())


def make_in_maps(inputs, n_cores, n_seq):
    g = lambda k: np.asarray(inputs[k], dtype=np.float32)
    x = g("x")
    bc = lambda v: np.ascontiguousarray(np.broadcast_to(v.reshape(1, D), (128, D)))
    w_in = g("w_in")[0]; w_out = g("w_out")[0]; wq = g("peer_wq")[0]
    cols = [w_in[:, 2560:3072], w_in[:, 0:512], w_in[:, 512:1024], w_in[:, 1024:1536], w_in[:, 1536:2048], w_in[:, 2048:2560],
            w_in[:, 3072:3584], w_out[:, 0:512], w_out[:, 512:1024]] + [wq[:, c * 512:(c + 1) * 512] for c in range(4)]
    wcat = np.ascontiguousarray(np.stack([c.reshape(8, 128, 512).transpose(1, 0, 2) for c in cols], axis=0)).reshape(13 * 128, 4096)
    common = {
        "gm_pk": np.ascontiguousarray(g("norm_mix")[0].reshape(8, 128).T), "gf_pk": np.ascontiguousarray(g("norm_ffn")[0].reshape(8, 128).T),
        "gfin_bc": bc(g("norm_f")),
        "wcat": wcat,
        "cw": np.ascontiguousarray(g("conv_w")[0].T.reshape(4, 128, 3).transpose(1, 0, 2)),
        "cgain": np.ascontiguousarray(g("conv_gain")[0].reshape(4, 128).T),
        "lbl": np.ascontiguousarray(g("hg_lb_logits").T.reshape(4, 128, 2).transpose(1, 0, 2)),
        "hgain": np.ascontiguousarray(g("hg_gain")[0].reshape(128, 1)),
        "kT": np.ascontiguousarray(g("peer_keys")[0].reshape(16, 128, 128).transpose(2, 0, 1)),
        "UT": np.ascontiguousarray(g("peer_u")[0].reshape(128 // GI, GI * 128, 8, 128).transpose(0, 3, 2, 1)).reshape(128 // GI * 128, 8 * GI * 128),
        "V": np.ascontiguousarray(g("peer_v")[0].reshape(128 // GI, GI, 128, D).transpose(0, 2, 1, 3)).reshape(128 // GI * 128, GI * D),
    }
    seq = x.shape[1]
    xs = x.reshape(n_cores, n_seq * seq, D)
    return [dict(common, x=np.ascontiguousarray(xs[c])) for c in range(n_cores)]


def kernel(**inputs):
    x = np.asarray(inputs["x"])
    B, Sq, _ = x.shape
    n_seq = B // N_CORES
    nc, _ = build_nc(n_seq, Sq)
    in_maps = make_in_maps(inputs, N_CORES, n_seq)
    res = run_bass_kernel_spmd(nc, in_maps, core_ids=list(range(N_CORES)))
    out = np.stack([np.asarray(r["out"]) for r in res.results], axis=0)
    return out.reshape(B, Sq, D).astype(np.float32)
```
